# Optimizing a Trainium2 kernel written in Bass

```python
import math
import jax, jax.numpy as jnp
from jax import lax
import numpy as np

D_MODEL = 1024
BATCH = 8
SEQ = 4096
DEPTH = 2

N_META = 16
D_CONV = D_MODEL // 2
CONV_WIDTH = 3
D_ATTN = D_MODEL - D_CONV
ATTN_HEAD_DIM = 64
N_ATTN_HEADS = D_ATTN // (2 * ATTN_HEAD_DIM)
QK_W = N_ATTN_HEADS * 2 * ATTN_HEAD_DIM
V_W = N_ATTN_HEADS * 2 * ATTN_HEAD_DIM
D_IN = 3 * D_CONV + 2 * QK_W + V_W
ROPE_THETA = 10000.0
Q_BLOCK = 128
D_FF_DENSE = 2816
N_EXPERTS = 8
TOP_K = 2
D_FF_EXPERT = 3584
N_DENSE_LAYERS = (DEPTH + 1) // 2
N_MOE_LAYERS = DEPTH // 2
LN_EPS = 1e-5
RMS_EPS = 1e-5
DEEPNORM_ALPHA = (2 * DEPTH) ** 0.25
DEEPNORM_BETA = (8 * DEPTH) ** -0.25

kernel_name = 'hybrid_conv_diffattn_deepnorm_moe'


def layer_norm(x, g, b):
    xf = x.astype(jnp.float32)
    mu = jnp.mean(xf, axis=-1, keepdims=True)
    xc = xf - mu
    var = jnp.mean(xc * xc, axis=-1, keepdims=True)
    y = xc * lax.rsqrt(var + LN_EPS) * g.astype(jnp.float32) + b.astype(jnp.float32)
    return y.astype(x.dtype)


def rms_norm(x, g):
    xf = x.astype(jnp.float32)
    y = xf * lax.rsqrt(jnp.mean(xf * xf, axis=-1, keepdims=True) + RMS_EPS) * g.astype(jnp.float32)
    return y.astype(x.dtype)


def rope_tables(length):
    inv = 1.0 / (ROPE_THETA ** (jnp.arange(0, ATTN_HEAD_DIM, 2, dtype=jnp.float32) / ATTN_HEAD_DIM))
    ang = jnp.arange(length, dtype=jnp.float32)[:, None] * inv[None, :]
    ang = jnp.concatenate([ang, ang], axis=-1)
    return jnp.cos(ang), jnp.sin(ang)


def apply_rope(x, cos, sin):
    c = cos[None, :, None, None, :].astype(x.dtype)
    s = sin[None, :, None, None, :].astype(x.dtype)
    x1, x2 = jnp.split(x, 2, axis=-1)
    return x * c + jnp.concatenate([-x2, x1], axis=-1) * s


def short_gated_conv(u_b, u_c, u_h, conv_w):
    v = u_c * u_h
    y = lax.conv_general_dilated(
        v, conv_w[:, None, :].astype(v.dtype), window_strides=(1,),
        padding=[(CONV_WIDTH - 1, 0)], dimension_numbers=('NWC', 'WIO', 'NWC'),
        feature_group_count=D_CONV)
    return u_b * y


def diff_attention(q, k, v, lam, lambda_init, subln_g, cos, sin):
    bsz, length = q.shape[0], q.shape[1]
    n_blk = -(-length // Q_BLOCK)
    lp = n_blk * Q_BLOCK
    pad = lp - length
    q = jnp.pad(apply_rope(q, cos, sin), ((0, 0), (0, pad), (0, 0), (0, 0), (0, 0)))
    k = jnp.pad(apply_rope(k, cos, sin), ((0, 0), (0, pad), (0, 0), (0, 0), (0, 0)))
    v = jnp.pad(v, ((0, 0), (0, pad), (0, 0), (0, 0)))
    q_blocks = q.reshape(bsz, n_blk, Q_BLOCK, N_ATTN_HEADS, 2, ATTN_HEAD_DIM).transpose(1, 0, 2, 3, 4, 5)
    kpos = jnp.arange(lp)
    scale = ATTN_HEAD_DIM ** -0.5

    def attend(args):
        qb, blk = args
        s = jnp.einsum('bqhcd,bkhcd->bhcqk', qb, k).astype(jnp.float32) * scale
        qpos = blk * Q_BLOCK + jnp.arange(Q_BLOCK)
        causal = kpos[None, :] <= qpos[:, None]
        p = jax.nn.softmax(jnp.where(causal, s, -jnp.inf), axis=-1)
        a = p[:, :, 0] - lam * p[:, :, 1]
        return jnp.einsum('bhqk,bkhe->bqhe', a.astype(v.dtype), v)

    o = lax.map(attend, (q_blocks, jnp.arange(n_blk)))
    o = o.transpose(1, 0, 2, 3, 4).reshape(bsz, lp, N_ATTN_HEADS, 2 * ATTN_HEAD_DIM)[:, :length]
    o = rms_norm(o, subln_g) * (1.0 - lambda_init)
    return o.reshape(bsz, length, D_ATTN)


def hybrid_mixer(h, w_in, conv_w, lq1, lk1, lq2, lk2, subln_g, w_out, lambda_init, cos, sin):
    bsz, length, _ = h.shape
    z = jnp.einsum('bld,de->ble', h, w_in)
    cuts = [D_CONV, 2 * D_CONV, 3 * D_CONV, 3 * D_CONV + QK_W, 3 * D_CONV + 2 * QK_W]
    u_b, u_c, u_h, q, k, v = jnp.split(z, cuts, axis=-1)
    conv_out = short_gated_conv(u_b, u_c, u_h, conv_w)
    q = q.reshape(bsz, length, N_ATTN_HEADS, 2, ATTN_HEAD_DIM)
    k = k.reshape(bsz, length, N_ATTN_HEADS, 2, ATTN_HEAD_DIM)
    v = v.reshape(bsz, length, N_ATTN_HEADS, 2 * ATTN_HEAD_DIM)
    lam = (jnp.exp(jnp.sum((lq1 * lk1).astype(jnp.float32)))
           - jnp.exp(jnp.sum((lq2 * lk2).astype(jnp.float32))) + lambda_init)
    attn_out = diff_attention(q, k, v, lam, lambda_init, subln_g, cos, sin)
    return jnp.einsum('ble,ed->bld', jnp.concatenate([conv_out, attn_out], axis=-1), w_out)


def swiglu(h, w_gate, w_up, w_down):
    return jnp.matmul(jax.nn.silu(jnp.matmul(h, w_gate)) * jnp.matmul(h, w_up), w_down)


def moe_swiglu(h, w_router, w_gate, w_up, w_down):
    logits = jnp.einsum('bld,de->ble', h, w_router).astype(jnp.float32)
    top_val, top_idx = lax.top_k(logits, TOP_K)
    top_w = jax.nn.softmax(top_val, axis=-1)
    gate = jnp.sum(jax.nn.one_hot(top_idx, N_EXPERTS, dtype=jnp.float32) * top_w[..., None], axis=-2)
    gate = gate.astype(h.dtype)
    out = jnp.zeros_like(h)
    for e in range(N_EXPERTS):
        out = out + gate[..., e:e + 1] * swiglu(h, w_gate[e], w_up[e], w_down[e])
    return out


def setup_inputs(seed: int = 0) -> dict:
    key = jax.random.key(seed)
    ks = jax.random.split(key, 26)
    f32 = jnp.float32

    def nrm(k, shape, s):
        return jax.random.normal(k, shape, f32) * s

    def gain(k, shape):
        return 1.0 + 0.01 * jax.random.normal(k, shape, f32)

    return {
        'x': nrm(ks[0], (BATCH, SEQ, D_MODEL), 1.0),
        'meta_tokens': nrm(ks[1], (N_META, D_MODEL), 1.0),
        'ln_emb_g': gain(ks[2], (D_MODEL,)),
        'ln_emb_b': nrm(ks[3], (D_MODEL,), 0.01),
        'w_in': nrm(ks[4], (DEPTH, D_MODEL, D_IN), D_MODEL ** -0.5),
        'conv_w': nrm(ks[5], (DEPTH, CONV_WIDTH, D_CONV), CONV_WIDTH ** -0.5),
        'lambda_q1': nrm(ks[6], (DEPTH, ATTN_HEAD_DIM), 0.1),
        'lambda_k1': nrm(ks[7], (DEPTH, ATTN_HEAD_DIM), 0.1),
        'lambda_q2': nrm(ks[8], (DEPTH, ATTN_HEAD_DIM), 0.1),
        'lambda_k2': nrm(ks[9], (DEPTH, ATTN_HEAD_DIM), 0.1),
        'subln_g': gain(ks[10], (DEPTH, 2 * ATTN_HEAD_DIM)),
        'w_out': nrm(ks[11], (DEPTH, D_MODEL, D_MODEL), D_MODEL ** -0.5 * DEEPNORM_BETA),
        'ln_mix_g': gain(ks[12], (DEPTH, D_MODEL)),
        'ln_mix_b': nrm(ks[13], (DEPTH, D_MODEL), 0.01),
        'ln_ffn_g': gain(ks[14], (DEPTH, D_MODEL)),
        'ln_ffn_b': nrm(ks[15], (DEPTH, D_MODEL), 0.01),
        'w_gate_dense': nrm(ks[16], (N_DENSE_LAYERS, D_MODEL, D_FF_DENSE), D_MODEL ** -0.5),
        'w_up_dense': nrm(ks[17], (N_DENSE_LAYERS, D_MODEL, D_FF_DENSE), D_MODEL ** -0.5),
        'w_down_dense': nrm(ks[18], (N_DENSE_LAYERS, D_FF_DENSE, D_MODEL), D_FF_DENSE ** -0.5 * DEEPNORM_BETA),
        'w_router': nrm(ks[19], (N_MOE_LAYERS, D_MODEL, N_EXPERTS), D_MODEL ** -0.5),
        'w_gate_moe': nrm(ks[20], (N_MOE_LAYERS, N_EXPERTS, D_MODEL, D_FF_EXPERT), D_MODEL ** -0.5),
        'w_up_moe': nrm(ks[21], (N_MOE_LAYERS, N_EXPERTS, D_MODEL, D_FF_EXPERT), D_MODEL ** -0.5),
        'w_down_moe': nrm(ks[22], (N_MOE_LAYERS, N_EXPERTS, D_FF_EXPERT, D_MODEL), D_FF_EXPERT ** -0.5 * DEEPNORM_BETA),
    }


def reference(x, meta_tokens, ln_emb_g, ln_emb_b, w_in, conv_w, lambda_q1, lambda_k1,
              lambda_q2, lambda_k2, subln_g, w_out, ln_mix_g, ln_mix_b, ln_ffn_g, ln_ffn_b,
              w_gate_dense, w_up_dense, w_down_dense, w_router, w_gate_moe, w_up_moe, w_down_moe):
    bsz = x.shape[0]
    meta = jnp.broadcast_to(meta_tokens[None].astype(x.dtype), (bsz, N_META, D_MODEL))
    h = jnp.concatenate([meta, x], axis=1)
    cos, sin = rope_tables(h.shape[1])
    h = layer_norm(h, ln_emb_g, ln_emb_b)
    for layer in range(DEPTH):
        lambda_init = 0.8 - 0.6 * math.exp(-0.3 * layer)
        mix = hybrid_mixer(h, w_in[layer], conv_w[layer], lambda_q1[layer], lambda_k1[layer],
                           lambda_q2[layer], lambda_k2[layer], subln_g[layer], w_out[layer],
                           lambda_init, cos, sin)
        h = layer_norm(DEEPNORM_ALPHA * h + mix, ln_mix_g[layer], ln_mix_b[layer])
        idx = layer // 2
        if layer % 2 == 0:
            ffn = swiglu(h, w_gate_dense[idx], w_up_dense[idx], w_down_dense[idx])
        else:
            ffn = moe_swiglu(h, w_router[idx], w_gate_moe[idx], w_up_moe[idx], w_down_moe[idx])
        h = layer_norm(DEEPNORM_ALPHA * h + ffn, ln_ffn_g[layer], ln_ffn_b[layer])
    return h[:, N_META:]
```

```python
import math
from contextlib import ExitStack

import numpy as np
import ml_dtypes

import concourse.bass as bass
import concourse.mybir as mybir
from concourse.bass_utils import run_bass_kernel_spmd

F32 = mybir.dt.float32
BF16 = mybir.dt.bfloat16
ALU = mybir.AluOpType
AF = mybir.ActivationFunctionType

D = 1024
SEQ = 4096
NMETA = 16
L = SEQ + NMETA
LP = 128 + SEQ
DEPTH = 2
FD = 2816
FE = 3584
NE = 8
ALPHA = (2 * DEPTH) ** 0.25
LN_EPS = 1e-5
NEG = -30000.0

ENGS = ("pe", "act", "dve", "pool", "sp")
I32 = mybir.dt.int32
SPARSE = True
NITEM = 23

DEBUG = False


class Op:
    __slots__ = ("eng", "fn", "deps", "is_dma", "sem", "val", "need_inc")

    def __init__(self, eng, fn, is_dma, sem):
        self.eng = eng
        self.fn = fn
        self.deps = []
        self.is_dma = is_dma
        self.sem = sem
        self.val = 0
        self.need_inc = is_dma


class Rec:
    def __init__(self):
        self.ops = {e: [] for e in ENGS}
        self.last_w = {}
        self.rd_eng = {}
        self.rd_dma = {}
        self.dma_cnt = {}
        self.pending = {}
        self.dmas_since_barrier = []

    def barrier(self):
        deps = []
        for e in ENGS:
            last = None
            for op in reversed(self.ops[e]):
                if not op.is_dma:
                    last = op
                    break
            if last is not None:
                deps.append(last)
        deps.extend(self.dmas_since_barrier)
        self.dmas_since_barrier = []
        for e in ENGS:
            self.pending[e] = list(deps)

    def _add(self, eng, fn, reads, writes, is_dma=False, sem=None):
        op = Op(eng, fn, is_dma, sem)
        psr = [k for k in reads if isinstance(k, tuple) and k and k[0] == "ps"]
        if psr:
            writes = list(writes) + [k for k in psr if k not in writes]
        deps = {}
        raw = set()
        for k in reads:
            w = self.last_w.get(k)
            if w is not None:
                deps[id(w)] = w
                raw.add(id(w))
        for k in writes:
            w = self.last_w.get(k)
            if w is not None:
                deps[id(w)] = w
            for r in self.rd_eng.get(k, {}).values():
                deps[id(r)] = r
            for r in self.rd_dma.get(k, ()):
                deps[id(r)] = r
        for d in self.pending.pop(eng, ()):
            deps[id(d)] = d
            raw.add(id(d))
        for d in deps.values():
            if d is op:
                continue
            if (not is_dma) and (not d.is_dma) and d.eng == eng:
                if eng == "pe" or id(d) not in raw:
                    continue
            op.deps.append((d, 16 * self.dma_cnt[d.sem] if d.is_dma else None))
            d.need_inc = True
        for k in reads:
            if is_dma:
                self.rd_dma.setdefault(k, []).append(op)
            else:
                self.rd_eng.setdefault(k, {})[eng] = op
        for k in writes:
            self.last_w[k] = op
            self.rd_eng[k] = {}
            self.rd_dma[k] = []
        if is_dma:
            self.dma_cnt[sem] = self.dma_cnt.get(sem, 0) + 1
            self.dmas_since_barrier.append(op)
        self.ops[eng].append(op)
        return op

    def op(self, eng, fn, reads=(), writes=()):
        return self._add(eng, fn, reads, writes)

    def dma(self, eng, fn, sem, reads=(), writes=(), background=False):
        self.dma_cnt.setdefault(sem, 0)
        op = self._add(eng, fn, reads, writes, is_dma=True, sem=sem)
        if background:
            self.dmas_since_barrier.pop()
        return op

    def emit(self, nc, final_wait_ops=()):
        cnt = {e: 0 for e in ENGS}
        for e in ENGS:
            for op in self.ops[e]:
                if (not op.is_dma) and op.need_inc:
                    cnt[e] += 1
                    op.val = cnt[e]
        with ExitStack() as st:
            esem = {e: st.enter_context(nc.semaphore("s_" + e)) for e in ENGS}
            dsem = {k: st.enter_context(nc.semaphore("d_%d" % i))
                    for i, k in enumerate(self.dma_cnt)}
            block = st.enter_context(nc.Block())
            beng = {"pe": block.tensor, "act": block.scalar, "dve": block.vector,
                    "pool": block.gpsimd, "sp": block.sync}

            def mk(e):
                def body(eng):
                    waited = {}
                    for op in self.ops[e]:
                        for d, dv in op.deps:
                            key = ("d", d.sem) if d.is_dma else ("e", d.eng)
                            v = dv if d.is_dma else d.val
                            if waited.get(key, 0) >= v:
                                continue
                            waited[key] = v
                            eng.wait_ge(dsem[d.sem] if d.is_dma else esem[d.eng], v)
                        ins = op.fn(eng)
                        if op.is_dma:
                            ins.then_inc(dsem[op.sem], 16)
                        elif op.need_inc:
                            ins.then_inc(esem[e], 1)
                    if e == "sp":
                        for sem_key in final_wait_ops:
                            eng.wait_ge(dsem[sem_key], 16 * self.dma_cnt[sem_key])
                return body

            for e in ENGS:
                beng[e](mk(e))
        return cnt


def token_blocks():
    blks = [(0, NMETA, 0)]
    for j in range(1, 9):
        blks.append((NMETA + 512 * (j - 1), 512, 128 + 512 * (j - 1)))
    return blks


class Builder:
    def __init__(self, debug=False):
        self.debug = debug
        self.nc = bass.Bass("TRN2", target_bir_lowering=False)
        self.R = Rec()
        self.st = ExitStack()
        self.bank_rr = 0
        self.uid = 0
        self.sbuf_bytes = 0

    def din(self, name, shape, dt=F32):
        return self.nc.dram_tensor(name, list(shape), dt, kind="ExternalInput").ap()

    def dout(self, name, shape, dt=F32):
        return self.nc.dram_tensor(name, list(shape), dt, kind="ExternalOutput").ap()

    def dscr(self, name, shape, dt):
        if self.debug:
            return self.nc.dram_tensor(name, list(shape), dt, kind="ExternalOutput").ap()
        return self.nc.dram_tensor(name, list(shape), dt).ap()

    def sb(self, stack, name, shape, dt):
        n = 1
        for s in shape[1:]:
            n *= s
        self.sbuf_bytes += n * (4 if dt == F32 else 2)
        self.uid += 1
        return stack.enter_context(self.nc.sbuf_tensor("sb%d_%s" % (self.uid, name), list(shape), dt))

    def bank(self, pool=None):
        pool = pool or (0, 1, 2, 3, 4, 5, 6, 7)
        b = pool[self.bank_rr % len(pool)]
        self.bank_rr += 1
        return b

    def mm(self, out, lhsT, rhs, start, stop, reads, writes):
        self.R.op("pe", lambda e: e.matmul(out, lhsT, rhs, start=start, stop=stop), reads, writes)

    def mmg(self, out, pairs, reads, writes, start=True, stop=True):
        pairs = list(pairs)

        def fn(e):
            ins = None
            n = len(pairs)
            for i, (l, r) in enumerate(pairs):
                ins = e.matmul(out, l, r, start=(start and i == 0), stop=(stop and i == n - 1))
            return ins
        self.R.op("pe", fn, reads, writes)

    def act(self, out, in_, func, reads, writes, scale=None, bias=None, accum_out=None):
        kw = {}
        if scale is not None:
            kw["scale"] = scale
        if bias is not None:
            kw["bias"] = bias
        if accum_out is not None:
            kw["accum_out"] = accum_out
        self.R.op("act", lambda e: e.activation(out=out, in_=in_, func=func, **kw), reads, writes)

    def tt(self, eng, out, in0, in1, op, reads, writes):
        self.R.op(eng, lambda e: e.tensor_tensor(out=out, in0=in0, in1=in1, op=op), reads, writes)

    def ts(self, eng, out, in0, s1, op0, reads, writes, s2=None, op1=None):
        if op1 is None:
            self.R.op(eng, lambda e: e.tensor_scalar(out=out, in0=in0, scalar1=s1, scalar2=None, op0=op0),
                      reads, writes)
        else:
            self.R.op(eng, lambda e: e.tensor_scalar(out=out, in0=in0, scalar1=s1, scalar2=s2, op0=op0, op1=op1),
                      reads, writes)

    def stt(self, out, in0, scalar, in1, op0, op1, reads, writes):
        self.R.op("dve", lambda e: e.scalar_tensor_tensor(out=out, in0=in0, scalar=scalar, in1=in1,
                                                           op0=op0, op1=op1), reads, writes)

    def cp(self, eng, out, in_, reads, writes):
        if eng == "act":
            self.act(out, in_, AF.Copy, reads, writes)
        else:
            self.R.op(eng, lambda e: e.tensor_copy(out=out, in_=in_), reads, writes)

    def dma(self, q, out, in_, sem, reads, writes, background=False):
        return self.R.dma(q, lambda e: e.dma_start(out=out, in_=in_), sem, reads, writes, background=background)


def build_program(debug=False, n_layers=DEPTH, stop_after=None, dbg_nexp=None, dbg_nblk=None):
    B = Builder(debug)
    nc, R = B.nc, B.R
    blks = token_blocks()

    x_d = B.din("x", [SEQ, D])
    meta_d = B.din("meta", [NMETA, D])
    embgb_d = B.din("embgb", [128, 2, 8])
    win_d = B.din("w_in_ext", [DEPTH, D, 4096])
    convw_d = B.din("convw", [DEPTH, 128, 4, 3])
    lam_d = B.din("lam", [DEPTH, 128, 4, 64])
    subg_d = B.din("subg", [DEPTH, 128, 1])
    wout_d = B.din("w_out", [DEPTH, D, D])
    lngb_d = B.din("lngb", [DEPTH, 128, 4, 8])
    wgd_d = B.din("w_gate_dense", [1, D, FD])
    wud_d = B.din("w_up_dense", [1, D, FD])
    wdd_d = B.din("w_down_dense", [1, FD, D])
    wr_d = B.din("w_router", [128, 8, NE])
    wgm_d = B.din("w_gate_moe", [1, NE, D, FE])
    wum_d = B.din("w_up_moe", [1, NE, D, FE])
    wdm_d = B.din("w_down_moe", [1, NE, FE, D])
    cos_d = B.din("cosT", [128, L])
    sin_d = B.din("sinT", [128, L])
    mask_d = B.din("maskb", [128, 4, 512], BF16)
    identf_d = B.din("identf", [128, 128])
    cb_d = B.din("constb", [128, 5, 128], BF16)
    sel_d = B.din("sel", [8, NE, 128])
    constf_d = B.din("constf", [128, 2, 128])
    gbrep_d = B.din("gbrep", [128, 2, D])
    y_d = B.dout("y", [SEQ, D])
    MTf = B.dscr("MTf", [SEQ, D], F32)
    MTb = B.dscr("MTb", [SEQ, D], BF16)
    Xg = B.dscr("Xg", [NITEM * 512, D], BF16)
    Ys = B.dscr("Ys", [NITEM * 512, D], F32)

    Hf = [B.dscr("Hf%d" % l, [128, 8, L], F32) for l in range(DEPTH)]
    Hb = [B.dscr("Hb%d" % l, [128, 8, L], BF16) for l in range(DEPTH)]
    Mf = [B.dscr("Mf%d" % l, [128, 8, L], F32) for l in range(DEPTH)]
    Mb = [B.dscr("Mb%d" % l, [128, 8, L], BF16) for l in range(DEPTH)]
    QS = [B.dscr("QS%d" % l, [128, 4, L], BF16) for l in range(DEPTH)]
    CO = [B.dscr("CO%d" % l, [128, 4, L], BF16) for l in range(DEPTH)]
    WGs = [B.nc.dram_tensor("WGs0", [1, D, FD], BF16).ap(), B.nc.dram_tensor("WGs1", [NE, D, FE], BF16).ap()]
    WUs = [B.nc.dram_tensor("WUs0", [1, D, FD], BF16).ap(), B.nc.dram_tensor("WUs1", [NE, D, FE], BF16).ap()]
    WDs = [B.nc.dram_tensor("WDs0", [1, FD, D], BF16).ap(), B.nc.dram_tensor("WDs1", [NE, FE, D], BF16).ap()]
    WG2, WU2, WD2 = WGs[1].rearrange("e d f -> (e d f)").rearrange("(r x) -> r x", x=4096), \
        WUs[1].rearrange("e d f -> (e d f)").rearrange("(r x) -> r x", x=4096), \
        WDs[1].rearrange("e f d -> (e f d)").rearrange("(r x) -> r x", x=4096)
    pconst_d = B.din("pconst", [128, 7])

    top = B.st
    ps = [top.enter_context(nc.psum_tensor("ps%d" % i, [128, 512], F32)) for i in range(8)]

    def PK(b):
        return ("ps", b)

    identf = B.sb(top, "identf", [128, 128], F32)
    cb = B.sb(top, "constb", [128, 5, 128], BF16)
    maskb = B.sb(top, "maskb", [128, 4, 512], BF16)
    embgb = B.sb(top, "embgb", [128, 2, 8], F32)
    lngb = B.sb(top, "lngb", [128, DEPTH, 4, 8], F32)
    convw = B.sb(top, "convw", [128, DEPTH, 4, 3], F32)
    lamt = B.sb(top, "lamt", [128, DEPTH, 4, 64], F32)
    subg = B.sb(top, "subg", [128, DEPTH, 1], F32)
    neglam = B.sb(top, "neglam", [128, DEPTH, 1], F32)
    gsc = B.sb(top, "gsc", [128, DEPTH, 1], F32)
    lamtmp = B.sb(top, "lamtmp", [128, 8], F32)
    lamprod = B.sb(top, "lamprod", [128, 2, 64], F32)
    wr = B.sb(top, "wr", [128, 8, NE], F32)
    sel = B.sb(top, "sel", [8, NE, 128], F32)
    constf = B.sb(top, "constf", [128, 2, 128], F32)
    LG = B.sb(top, "LG", [128, 32, NE], F32)
    B.dma("sp", constf[:], constf_d, "c0", [], ["constf"])
    epst = B.sb(top, "epst", [128, 1], F32)
    R.op("pool", lambda e: e.memset(epst[:], LN_EPS), [], ["epst"])
    identb = cb[:, 0, :]
    onesb = cb[:, 1, :]
    ones0b = cb[:, 2, :]
    onesM = cb[:, 3, :]
    ones128 = cb[:, 4, :]

    B.dma("sp", identf[:], identf_d, "c0", [], ["identf"])
    B.dma("sp", cb[:], cb_d, "c0", [], ["cb"])
    B.dma("sp", maskb[:], mask_d, "c0", [], ["maskb"])
    B.dma("sp", embgb[:], embgb_d, "c0", [], ["embgb"])
    for l in range(DEPTH):
        B.dma("sp", lngb[:, l], lngb_d[l], "c0", [], ["lngb"])
        B.dma("sp", convw[:, l], convw_d[l], "c0", [], ["convw"])
        B.dma("sp", lamt[:, l], lam_d[l], "c0", [], ["lamt"])
        B.dma("sp", subg[:, l], subg_d[l], "c0", [], ["subg"])
    B.dma("sp", wr[:], wr_d, "c0", [], ["wr"])
    B.dma("sp", sel[:], sel_d, "c0", [], ["sel"])

    def cast(name, dst, src):
        c = src.shape[-1]
        parts = 1
        while c // parts > 2048 or c % parts:
            parts += 1
        if len(src.shape) == 3:
            src = src.rearrange("e r (a b) -> (e r a) b", a=parts)
            dst = dst.rearrange("e r (a b) -> (e r a) b", a=parts)
        else:
            src = src.rearrange("r (a b) -> (r a) b", a=parts)
            dst = dst.rearrange("r (a b) -> (r a) b", a=parts)
        B.dma("pool", dst, src, ("cast", name), [], [("W", name)], background=True)

    WGd2 = WGs[0].rearrange("e d f -> (e d f)").rearrange("(r x) -> r x", x=2048)
    WUd2 = WUs[0].rearrange("e d f -> (e d f)").rearrange("(r x) -> r x", x=2048)
    WDd2 = WDs[0].rearrange("e f d -> (e f d)").rearrange("(r x) -> r x", x=2048)
    dense_cast_keys = []
    dense_cast_queue = []

    def issue_dense_casts():
        for nm, dst2, srcw in (("g", WGd2, wgd_d), ("u", WUd2, wud_d)):
            for kc in range(8):
                dst = dst2[:, kc * 256:(kc + 1) * 256].rearrange("(g p) c -> p g c", p=128)
                src = srcw[0, kc * 128:(kc + 1) * 128, :].rearrange("p (g c) -> p g c", c=256)
                dense_cast_keys.append(("W0", nm, kc))
                dense_cast_queue.append(lambda dst=dst, src=src, sem=("cast", nm + "0"), key=dense_cast_keys[-1]:
                                        B.dma("pool", dst, src, sem, [], [key], background=True))
        for c in range(2):
            dst = WDd2[:, c * 1024:(c + 1) * 1024].rearrange("(g p) d -> p g d", p=128)
            src = wdd_d[0].rearrange("(g c p) d -> c p g d", p=128, c=2)[c]
            dense_cast_keys.append(("W0", "d", c))
            dense_cast_queue.append(lambda dst=dst, src=src, key=dense_cast_keys[-1]:
                                    B.dma("pool", dst, src, ("cast", "d0"), [], [key], background=True))

    moe_cast_keys = []
    cast_queue = []

    def pump_casts(k):
        for _ in range(k):
            if cast_queue:
                cast_queue.pop(0)()

    def issue_moe_cast(h):
        if not SPARSE:
            cast("g1_%d" % h, WGs[1][2 * h:2 * h + 2], wgm_d[0, 2 * h:2 * h + 2])
            cast("u1_%d" % h, WUs[1][2 * h:2 * h + 2], wum_d[0, 2 * h:2 * h + 2])
            cast("d1_%d" % h, WDs[1][2 * h:2 * h + 2], wdm_d[0, 2 * h:2 * h + 2])
            return
        for e_ in (2 * h, 2 * h + 1):
            for nm, dst2, srcw in (("g", WG2, wgm_d), ("u", WU2, wum_d)):
                for kc in range(8):
                    dst = dst2[e_ * 896:(e_ + 1) * 896, kc * 512:(kc + 1) * 512].rearrange("(g p) c -> p g c", p=128)
                    src = srcw[0, e_, kc * 128:(kc + 1) * 128, :].rearrange("p (g c) -> p g c", c=512)
                    moe_cast_keys.append(("W2", nm, e_, kc))
                    cast_queue.append(lambda dst=dst, src=src, sem=("cast", "%s1_%d" % (nm, h)), key=moe_cast_keys[-1]:
                                      B.dma("pool", dst, src, sem, [], [key], background=True))
            for c in range(4):
                dst = WD2[e_ * 896:(e_ + 1) * 896, c * 1024:(c + 1) * 1024].rearrange("(g p) d -> p g d", p=128)
                src = wdm_d[0, e_].rearrange("(g c p) d -> c p g d", p=128, c=4)[c]
                moe_cast_keys.append(("W2", "d", e_, c))
                cast_queue.append(lambda dst=dst, src=src, sem=("cast", "d1_%d" % h), key=moe_cast_keys[-1]:
                                  B.dma("pool", dst, src, sem, [], [key], background=True))

    for l in range(n_layers):
        lam_init = 0.8 - 0.6 * math.exp(-0.3 * l)
        B.tt("dve", lamprod[:, 0, :], lamt[:, l, 0, :], lamt[:, l, 1, :], ALU.mult, ["lamt"], ["lamprod"])
        B.tt("dve", lamprod[:, 1, :], lamt[:, l, 2, :], lamt[:, l, 3, :], ALU.mult, ["lamt"], ["lamprod2"])
        R.op("dve", lambda e: e.reduce_sum(out=lamtmp[:, 0:1], in_=lamprod[:, 0, :], axis=mybir.AxisListType.X),
             ["lamprod"], ["lt0"])
        R.op("dve", lambda e: e.reduce_sum(out=lamtmp[:, 1:2], in_=lamprod[:, 1, :], axis=mybir.AxisListType.X),
             ["lamprod2"], ["lt1"])
        B.act(lamtmp[:, 2:4], lamtmp[:, 0:2], AF.Exp, ["lt0", "lt1"], ["lt2"])
        B.tt("dve", lamtmp[:, 4:5], lamtmp[:, 3:4], lamtmp[:, 2:3], ALU.subtract, ["lt2"], ["lt4"])
        B.ts("dve", neglam[:, l, :], lamtmp[:, 4:5], -lam_init, ALU.add, ["lt4"], [("neglam", l)])
        B.ts("dve", gsc[:, l, :], subg[:, l, :], 1.0 - lam_init, ALU.mult, ["subg"], [("gsc", l)])

    def ln_stats_dc(bm, bq, dc, n, x, xbs, xsqs):
        u = dc % 2
        B.cp("dve", xbs[:, u, :n], x[:, dc, :n], [("x", dc)], [("xbs", u)])
        B.act(xsqs[:, u, :n], x[:, dc, :n], AF.Square, [("x", dc)], [("xsqs", u)])
        B.mm(ps[bm][:, :n], onesM, xbs[:, u, :n], dc == 0, dc == 7, ["cb", ("xbs", u)], [PK(bm)])
        B.mm(ps[bq][:, :n], onesM, xsqs[:, u, :n], dc == 0, dc == 7, ["cb", ("xsqs", u)], [PK(bq)])

    def ln_finish(bm, bq, n, x, tmp, small, gcol, bcol, ob, ob_key):
        m2, var, rstd, nmr = (small[:, i, :n] for i in range(4))
        B.act(m2, ps[bm][:, :n], AF.Square, [PK(bm)], ["m2"])
        B.stt(var, ps[bq][:, :n], LN_EPS, m2, ALU.add, ALU.subtract, [PK(bq), "m2"], ["var"])
        B.act(var, var, AF.Ln, ["var"], ["var"])
        B.act(rstd, var, AF.Exp, ["var"], ["rstd"], scale=-0.5)
        B.stt(nmr, ps[bm][:, :n], -1.0, rstd, ALU.mult, ALU.mult, [PK(bm), "rstd"], ["nmr"])
        for dc in range(8):
            t = tmp[:, dc % 2, :n]
            tk = ("lnt", dc % 2)
            B.tt("dve", t, x[:, dc, :n], rstd, ALU.mult, [("x", dc), "rstd"], [tk])
            B.tt("pool", t, t, nmr, ALU.add, [tk, "nmr"], [tk])
            B.act(x[:, dc, :n], t, AF.Identity, [tk, "lngb"], [("x", dc)], scale=gcol(dc), bias=bcol(dc))
            if ob is not None:
                B.act(ob[:, dc, :n], t, AF.Identity, [tk, "lngb"], [(ob_key, dc)], scale=gcol(dc), bias=bcol(dc))

    with ExitStack() as es:
        NES = 4
        xt = [B.sb(es, "e_xt%d" % i, [128, D], F32) for i in range(NES)]
        xn = [B.sb(es, "e_xn%d" % i, [128, D], F32) for i in range(NES)]
        st6 = B.sb(es, "e_st6", [128, NES, 2, 6], F32)
        mv = B.sb(es, "e_mv", [128, NES, 4], F32)
        hfo = [B.sb(es, "e_hf%d" % i, [128, 8, 128], F32) for i in range(NES)]
        hbo = [B.sb(es, "e_hb%d" % i, [128, 8, 128], BF16) for i in range(NES)]
        issue_dense_casts()
        def e_geom(i):
            return i % NES, (NMETA if i == 0 else 128), (0 if i == 0 else NMETA + (i - 1) * 128)

        def e_stage1(i):
            s, r, t0 = e_geom(i)
            src = meta_d if i == 0 else x_d[(i - 1) * 128:i * 128, :]
            B.dma("sp", xt[s][:r, :], src, ("e_xt", s), [], [("e_xt", s)])
            for h in range(2):
                R.op("dve", lambda e, s=s, h=h, r=r: e.bn_stats(out=st6[:r, s, h, :], in_=xt[s][:r, h * 512:(h + 1) * 512]),
                     [("e_xt", s)], [("e_st", s, h)])
            R.op("dve", lambda e, s=s, r=r: e.bn_aggr(out=mv[:r, s, 0:2], in_=st6[:r, s].rearrange("p a b -> p (a b)")),
                 [("e_st", s, 0), ("e_st", s, 1)], [("e_mv", s)])
            B.ts("dve", mv[:r, s, 3:4], mv[:r, s, 1:2], LN_EPS, ALU.add, [("e_mv", s)], [("e_ve", s)])
            B.act(mv[:r, s, 3:4], mv[:r, s, 3:4], AF.Ln, [("e_ve", s)], [("e_ve", s)])
            B.act(mv[:r, s, 2:3], mv[:r, s, 3:4], AF.Exp, [("e_ve", s)], [("e_rs", s)], scale=-0.5)

        def e_stage2(i):
            s, r, t0 = e_geom(i)
            B.ts("dve", xn[s][:r, :], xt[s][:r, :], mv[:r, s, 0:1], ALU.subtract,
                 [("e_xt", s), ("e_mv", s), ("e_rs", s)], [("e_xn", s)], s2=mv[:r, s, 2:3], op1=ALU.mult)
            for half in range(2):
                bk = B.bank()

                def tr(e, s=s, r=r, half=half, bk=bk):
                    ins = None
                    for q in range(4):
                        dc = half * 4 + q
                        ins = e.transpose(ps[bk][:, q * 128:q * 128 + r], xn[s][:r, dc * 128:(dc + 1) * 128],
                                          identf[:r, :r])
                    return ins
                R.op("pe", tr, [("e_xn", s), "identf"], [PK(bk)])
                for q in range(4):
                    dc = half * 4 + q
                    B.act(hfo[s][:, dc, :r], ps[bk][:, q * 128:q * 128 + r], AF.Identity,
                          [PK(bk), "embgb"], [("e_hf", s, dc)], scale=embgb[:, 0, dc:dc + 1], bias=embgb[:, 1, dc:dc + 1])

        def e_stage3(i):
            s, r, t0 = e_geom(i)
            B.cp("pool", hbo[s][:, :, :r], hfo[s][:, :, :r], [("e_hf", s, dc) for dc in range(8)], [("e_hb", s)])
            B.dma("sp", Hf[0][:, :, t0:t0 + r], hfo[s][:, :, :r], ("e_of", s),
                  [("e_hf", s, dc) for dc in range(8)], [("Hf", 0, "t", i)])
            B.dma("sp", Hb[0][:, :, t0:t0 + r], hbo[s][:, :, :r], ("e_of", s), [("e_hb", s)], [("Hb", 0, "t", i)])

        for step in range(33 + 2):
            if step < 33:
                e_stage1(step)
            if 0 <= step - 1 < 33:
                e_stage2(step - 1)
            if 0 <= step - 2 < 33:
                e_stage3(step - 2)

    R.barrier()

    def hkeys(name, l, j):
        if l == 0:
            if j == 0:
                return [(name, 0, "t", 0)]
            return [(name, 0, "t", 4 * (j - 1) + 1 + q) for q in range(4)]
        return [(name, l, j)]

    final_ops = []
    moe_cast_next = [0]
    g01 = B.sb(top, "g01", [128, 2, 32], F32)
    gidx = B.sb(top, "gidx", [128, 2, 32], I32)
    eidx = B.sb(top, "eidx", [128, 32], I32)
    widx = B.sb(top, "widx", [128, NITEM, 7], I32)
    pconst = B.sb(top, "pconst", [128, 7], F32)
    B.dma("sp", pconst[:], pconst_d, "c0", [], ["pconst"])
    if debug:
        dbg_gidx = B.nc.dram_tensor("dbg_gidx", [128, 2, 32], I32, kind="ExternalOutput").ap()
        dbg_eidx = B.nc.dram_tensor("dbg_eidx", [128, 32], I32, kind="ExternalOutput").ap()
        dbg_g01 = B.nc.dram_tensor("dbg_g01", [128, 2, 32], F32, kind="ExternalOutput").ap()

    def sparse_moe_phase(l):
        AX = mybir.AxisListType.X
        FC = FE // 128
        GC = 512
        NG = FE // GC
        CPG = GC // 128
        DG = 4
        NDG = FC // DG
        castkeys = list(moe_cast_keys)
        onesf = constf[:, 0, :]
        utri = constf[:, 1, :]
        with ExitStack() as es:
            m8 = B.sb(es, "r_m8", [128, 32, NE], F32)
            selt = B.sb(es, "r_sel", [128, 32, NE], F32)
            top1 = B.sb(es, "r_top1", [128, 32, NE], F32)
            cum = B.sb(es, "r_cum", [128, 32, NE], F32)
            tot = B.sb(es, "r_tot", [128, 32, NE], F32)
            off = B.sb(es, "r_off", [128, 32, NE], F32)
            gs = B.sb(es, "r_gs", [128, 32, NE], F32)
            prod = B.sb(es, "r_prod", [128, 32, NE], F32)
            sm = B.sb(es, "r_sm", [128, 8, NE], F32)
            gk = B.sb(es, "r_gk", [128, 2, 32], F32)
            dd = B.sb(es, "r_dd", [128, 3, 32], F32)
            eid = B.sb(es, "r_eid", [128, 32], F32)
            for t in range(32):
                R.op("dve", lambda e, t=t: e.max(out=m8[:, t, :], in_=LG[:, t, :]), [("LG", t)], [("m8", t)])
            for t in range(32):
                B.ts("dve", selt[:, t, :], LG[:, t, :], m8[:, t, 1:2], ALU.is_ge, [("LG", t), ("m8", t)], ["r_sel"])
                B.ts("dve", top1[:, t, :], LG[:, t, :], m8[:, t, 0:1], ALU.is_ge, [("LG", t), ("m8", t)], ["r_top1"])
            m8k = [("m8", t) for t in range(32)]
            B.tt("dve", dd[:, 0, :], m8[:, :, 1], m8[:, :, 0], ALU.subtract, m8k, ["r_d0"])
            B.act(dd[:, 1, :], dd[:, 0, :], AF.Exp, ["r_d0"], ["r_d1"])
            B.ts("dve", dd[:, 2, :], dd[:, 1, :], 1.0, ALU.add, ["r_d1"], ["r_d2"])
            R.op("dve", lambda e: e.reciprocal(out=g01[:, 0, :], in_=dd[:, 2, :]), ["r_d2"], ["g0"])
            B.tt("dve", g01[:, 1, :], dd[:, 1, :], g01[:, 0, :], ALU.mult, ["r_d1", "g0"], ["g1"])
            selflat = selt[:].rearrange("p t e -> p (t e)")
            b1, b2 = B.bank(), B.bank()
            B.mm(ps[b1][:, 0:256], utri, selflat, True, True, ["constf", "r_sel"], [PK(b1)])
            B.mm(ps[b2][:, 0:256], onesf, selflat, True, True, ["constf", "r_sel"], [PK(b2)])
            B.cp("dve", cum[:].rearrange("p t e -> p (t e)"), ps[b1][:, 0:256], [PK(b1)], ["r_cum"])
            B.cp("act", tot[:].rearrange("p t e -> p (t e)"), ps[b2][:, 0:256], [PK(b2)], ["r_tot"])
            R.op("dve", lambda e: e.memset(off[:, 0, :], 0.0), [], [("r_off", 0)])
            for t in range(1, 32):
                B.tt("dve", off[:, t, :], off[:, t - 1, :], tot[:, t - 1, :], ALU.add, [("r_off", t - 1), "r_tot"], [("r_off", t)])
            cnt, ntile, ibase, iend, base5, tq = (sm[:, i, :] for i in range(6))
            B.tt("dve", cnt, off[:, 31, :], tot[:, 31, :], ALU.add, [("r_off", 31), "r_tot"], ["r_cnt"])
            B.ts("dve", ntile, cnt, 0.0, ALU.is_gt, ["r_cnt"], ["r_nt"])
            for m in range(1, 8):
                B.ts("dve", tq, cnt, 512.0 * m, ALU.is_gt, ["r_cnt"], ["r_tq"])
                B.tt("dve", ntile, ntile, tq, ALU.add, ["r_nt", "r_tq"], ["r_nt"])
            R.op("dve", lambda e: e.memset(ibase[:, 0:1], 0.0), [], [("r_ib", 0)])
            for e_ in range(1, NE):
                B.tt("dve", ibase[:, e_:e_ + 1], ibase[:, e_ - 1:e_], ntile[:, e_ - 1:e_], ALU.add,
                     [("r_ib", e_ - 1), "r_nt"], [("r_ib", e_)])
            ibk = [("r_ib", e_) for e_ in range(NE)]
            B.tt("dve", iend, ibase, ntile, ALU.add, ibk + ["r_nt"], ["r_ie"])
            B.ts("dve", base5, ibase, 512.0, ALU.mult, ibk, ["r_b5"])
            for t in range(32):
                B.tt("dve", off[:, t, :], off[:, t, :], base5, ALU.add, [("r_off", t), "r_b5"], [("r_off2", t)])
            offk = [("r_off", t) for t in range(32)] + [("r_off2", t) for t in range(32)]
            B.tt("dve", gs[:], cum[:], off[:], ALU.add, ["r_cum"] + offk, ["r_gs"])
            B.tt("dve", prod[:], top1[:], gs[:], ALU.mult, ["r_top1", "r_gs"], ["r_prod"])
            R.op("dve", lambda e: e.reduce_sum(out=gk[:, 0, :], in_=prod[:], axis=AX), ["r_prod"], ["r_gk0"])
            B.tt("dve", top1[:], selt[:], top1[:], ALU.subtract, ["r_sel", "r_top1"], ["r_top2"])
            B.tt("dve", prod[:], top1[:], gs[:], ALU.mult, ["r_top2", "r_gs", "r_gk0"], ["r_prod"])
            R.op("dve", lambda e: e.reduce_sum(out=gk[:, 1, :], in_=prod[:], axis=AX), ["r_prod"], ["r_gk1"])
            B.cp("dve", gidx[:], gk[:], ["r_gk0", "r_gk1"], ["gidx"])
            for w in range(NITEM):
                B.ts("dve", tq, iend, float(w), ALU.is_le, ["r_ie"], ["r_tq"])
                R.op("dve", lambda e, w=w: e.reduce_sum(out=eid[:, w:w + 1], in_=tq, axis=AX), ["r_tq"], [("r_eid", w)])
            eidk = [("r_eid", w) for w in range(NITEM)]
            B.ts("dve", eid[:, 0:NITEM], eid[:, 0:NITEM], float(NE - 1), ALU.min, eidk, ["r_eid2"], s2=896.0, op1=ALU.mult)
            B.cp("dve", eidx[:, 0:NITEM], eid[:, 0:NITEM], ["r_eid2"], ["eidx"])
            wif = B.sb(es, "r_wif", [128, NITEM, 7], F32)
            for w in range(NITEM):
                B.ts("dve", wif[:, w, :], pconst[:, :], eid[:, w:w + 1], ALU.add, ["r_eid2", "pconst"], [("r_wif", w)])
            B.cp("dve", widx[:], wif[:], [("r_wif", w) for w in range(NITEM)], ["widx"])
            if debug:
                B.dma("sp", dbg_gidx, gidx[:], "dbg1", ["gidx"], [])
                B.dma("sp", dbg_eidx, eidx[:], "dbg1", ["eidx"], [])
                B.dma("sp", dbg_g01, g01[:], "dbg1", ["g0", "g1"], [])
        R.barrier()
        if stop_after == ("R", l):
            return
        with ExitStack() as es:
            mt = [B.sb(es, "s_mt%d" % i, [128, D], BF16) for i in range(4)]
            xgz = [("Xgz", w_) for w_ in range(NITEM)]
            for tt in range(32):
                s = tt % 4
                B.dma("sp", mt[s][:], MTb[tt * 128:(tt + 1) * 128, :], ("s_mt", s), [("MTb", tt)], [("s_mt", s)])
                for k in range(2):
                    R.dma("pool", lambda e, s=s, k=k, tt=tt: e.indirect_dma_start(
                        out=Xg, out_offset=bass.IndirectOffsetOnAxis(ap=gidx[:, k, tt:tt + 1], axis=0),
                        in_=mt[s][:], in_offset=None), ("s_sc", s), [("s_mt", s), "gidx"] + xgz, [("Xgw", tt, k)])
        R.barrier()
        if stop_after == ("S", l):
            return
        with ExitStack() as es:
            NWB = 2
            wgu = [B.sb(es, "m_wgu%d" % i, [128, 2, 8, GC], BF16) for i in range(NWB)]
            wdt = [B.sb(es, "m_wd%d" % i, [128, DG, D], BF16) for i in range(NWB)]
            xgt = [B.sb(es, "m_xg%d" % i, [128, 4, D], BF16) for i in range(2)]
            xT = [B.sb(es, "m_xT%d" % i, [128, 8, 512], BF16) for i in range(2)]
            actt = B.sb(es, "m_act", [128, FC, 512], BF16)
            sg = [B.sb(es, "m_sg%d" % i, [128, 512], F32) for i in range(3)]
            ysb = [B.sb(es, "m_y%d" % i, [128, 4, D], F32) for i in range(2)]
            xgw = [("Xgw", tt, k) for tt in range(32) for k in range(2)]
            wc = dwc = sgc = 0
            def item_inputs(w):
                s = w % 2
                B.dma("sp", xgt[s][:], Xg[w * 512:(w + 1) * 512, :].rearrange("(q p) d -> p q d", p=128),
                      ("m_xg", s), xgw, [("m_xg", s)])
                for kp in range(4):
                    bk = B.bank()
                    psb = ps[bk][:].bitcast(BF16)

                    def trx(e, s=s, kp=kp, psb=psb):
                        ins = None
                        for k2 in range(2):
                            kc = 2 * kp + k2
                            for q in range(4):
                                ins = e.transpose(psb[:, k2 * 512 + q * 128:k2 * 512 + (q + 1) * 128],
                                                  xgt[s][:, q, kc * 128:(kc + 1) * 128], identb)
                        return ins
                    R.op("pe", trx, [("m_xg", s), "cb"], [PK(bk)])
                    for k2 in range(2):
                        kc = 2 * kp + k2
                        B.cp("act" if k2 == 0 else "dve", xT[s][:, kc, :], psb[:, k2 * 512:(k2 + 1) * 512],
                             [PK(bk)], [("m_xT", s, kc)])

            item_inputs(0)
            for w in range(NITEM):
                s = w % 2
                xTk = [("m_xT", s, kc) for kc in range(8)]

                def gath(out_ap, src2, g, w=w):
                    return lambda e: e.indirect_dma_start(
                        out=out_ap, out_offset=None, in_=src2,
                        in_offset=bass.IndirectOffsetOnAxis(ap=widx[:, w, g:g + 1], axis=0))
                for g in range(NG):
                    if g == 3 and w + 1 < NITEM:
                        item_inputs(w + 1)
                    wi = wc % NWB
                    wc += 1
                    R.dma("pool", gath(wgu[wi][:, 0, :, :].rearrange("p k c -> p (k c)"), WG2, g),
                          ("m_wgu", wi), ["widx"] + castkeys, [("m_wgu", wi)])
                    R.dma("pool", gath(wgu[wi][:, 1, :, :].rearrange("p k c -> p (k c)"), WU2, g),
                          ("m_wgu", wi), ["widx"] + castkeys, [("m_wgu", wi)])
                    for c in range(CPG):
                        fc = g * CPG + c
                        bg, bu = B.bank(), B.bank()
                        B.mmg(ps[bg][:, :], [(wgu[wi][:, 0, kc, c * 128:(c + 1) * 128], xT[s][:, kc, :]) for kc in range(8)],
                              [("m_wgu", wi)] + xTk, [PK(bg)])
                        B.mmg(ps[bu][:, :], [(wgu[wi][:, 1, kc, c * 128:(c + 1) * 128], xT[s][:, kc, :]) for kc in range(8)],
                              [("m_wgu", wi)] + xTk, [PK(bu)])
                        q_ = sgc % 3
                        sgc += 1
                        B.act(sg[q_][:, :], ps[bg][:, :], AF.Silu, [PK(bg)], [("m_sg", q_)])
                        B.tt("dve", actt[:, fc, :], ps[bu][:, :], sg[q_][:, :], ALU.mult,
                             [PK(bu), ("m_sg", q_)], [("m_act", fc)])
                for dg in range(NDG):
                    wi = dwc % NWB
                    dwc += 1
                    R.dma("pool", gath(wdt[wi][:].rearrange("p c d -> p (c d)"), WD2, dg),
                          ("m_wd", wi), ["widx"] + castkeys, [("m_wd", wi)])
                    for qh in range(8):
                        q, half = qh // 2, qh % 2
                        for c in range(DG):
                            fc = dg * DG + c
                            B.mm(ps[qh][:, :], actt[:, fc, q * 128:(q + 1) * 128], wdt[wi][:, c, half * 512:(half + 1) * 512],
                                 fc == 0, fc == FC - 1, [("m_wd", wi), ("m_act", fc)], [PK(qh)])
                for qh in range(8):
                    q, half = qh // 2, qh % 2
                    B.cp("act" if qh % 2 == 0 else "dve", ysb[s][:, q, half * 512:(half + 1) * 512], ps[qh][:, :],
                         [PK(qh)], [("m_y", s, qh)])
                B.bank_rr = 0
                B.dma("sp", Ys[w * 512:(w + 1) * 512, :].rearrange("(q p) d -> p q d", p=128), ysb[s][:], ("m_ys", s),
                      [("m_y", s, qh) for qh in range(8)], [("Ys", w)])
        R.barrier()
        if stop_after == ("I", l):
            return
        with ExitStack() as es:
            gb = B.sb(es, "f_gb", [128, 2, D], F32)
            B.dma("sp", gb[:], gbrep_d, "f_gb", [], ["f_gb"])
            NS = 4
            y0 = [B.sb(es, "f_y0_%d" % i, [128, D], F32) for i in range(NS)]
            y1 = [B.sb(es, "f_y1_%d" % i, [128, D], F32) for i in range(NS)]
            hm = [B.sb(es, "f_hm%d" % i, [128, D], F32) for i in range(NS)]
            yo = [B.sb(es, "f_yo%d" % i, [128, D], F32) for i in range(NS)]
            st6 = B.sb(es, "f_st6", [128, NS, 2, 6], F32)
            mv = B.sb(es, "f_mv", [128, NS, 4], F32)
            ysk = [("Ys", w) for w in range(NITEM)]
            def f_stage1(tt):
                s = tt % NS
                R.dma("pool", lambda e, s=s, tt=tt: e.indirect_dma_start(
                    out=y0[s][:], out_offset=None, in_=Ys,
                    in_offset=bass.IndirectOffsetOnAxis(ap=gidx[:, 0, tt:tt + 1], axis=0)),
                    ("f_g0", s), ysk + ["gidx"], [("f_y0", s)])
                R.dma("pool", lambda e, s=s, tt=tt: e.indirect_dma_start(
                    out=y1[s][:], out_offset=None, in_=Ys,
                    in_offset=bass.IndirectOffsetOnAxis(ap=gidx[:, 1, tt:tt + 1], axis=0)),
                    ("f_g1", s), ysk + ["gidx"], [("f_y1", s)])
                B.dma("sp", hm[s][:], MTf[tt * 128:(tt + 1) * 128, :], ("f_hm", s), [("MTf", tt)], [("f_hm", s)])
                B.act(hm[s][:], hm[s][:], AF.Copy, [("f_hm", s)], [("f_hm", s)], scale=ALPHA)

            def f_stage2(tt):
                s = tt % NS
                B.stt(y0[s][:], y0[s][:], g01[:, 0, tt:tt + 1], hm[s][:], ALU.mult, ALU.add,
                      [("f_y0", s), ("f_hm", s), "g0"], [("f_y0", s)])
                B.stt(y0[s][:], y1[s][:], g01[:, 1, tt:tt + 1], y0[s][:], ALU.mult, ALU.add,
                      [("f_y1", s), ("f_y0", s), "g1"], [("f_y0", s)])
                for h in range(2):
                    R.op("dve", lambda e, s=s, h=h: e.bn_stats(out=st6[:, s, h, :], in_=y0[s][:, h * 512:(h + 1) * 512]),
                         [("f_y0", s)], [("f_st", s, h)])
                R.op("dve", lambda e, s=s: e.bn_aggr(out=mv[:, s, 0:2], in_=st6[:, s].rearrange("p a b -> p (a b)")),
                     [("f_st", s, 0), ("f_st", s, 1)], [("f_mv", s)])
                B.ts("dve", mv[:, s, 3:4], mv[:, s, 1:2], LN_EPS, ALU.add, [("f_mv", s)], [("f_ve", s)])
                B.act(mv[:, s, 3:4], mv[:, s, 3:4], AF.Ln, [("f_ve", s)], [("f_ve", s)])
                B.act(mv[:, s, 2:3], mv[:, s, 3:4], AF.Exp, [("f_ve", s)], [("f_rs", s)], scale=-0.5)

            def f_stage3(tt):
                s = tt % NS
                B.ts("dve", yo[s][:], y0[s][:], mv[:, s, 0:1], ALU.subtract,
                     [("f_y0", s), ("f_mv", s), ("f_rs", s)], [("f_yo", s)], s2=mv[:, s, 2:3], op1=ALU.mult)
                B.tt("dve", yo[s][:], yo[s][:], gb[:, 0, :], ALU.mult, [("f_yo", s), "f_gb"], [("f_yo", s)])
                B.tt("pool", yo[s][:], yo[s][:], gb[:, 1, :], ALU.add, [("f_yo", s), "f_gb"], [("f_yo", s)])
                final_ops.append(B.dma("sp", y_d[tt * 128:(tt + 1) * 128, :], yo[s][:], ("c_y", s), [("f_yo", s)], []))

            for step in range(32 + 2):
                if step < 32:
                    f_stage1(step)
                if 0 <= step - 1 < 32:
                    f_stage2(step - 1)
                if 0 <= step - 2 < 32:
                    f_stage3(step - 2)

    for l in range(n_layers):
        last = (l == DEPTH - 1)
        moe = (l % 2 == 1)
        with ExitStack() as ls:
            KT = B.sb(ls, "KT", [128, 4, LP], BF16)
            V = B.sb(ls, "V", [128, 33, 512], BF16)
            if l == 0 or True:
                R.op("pool", lambda e, KT=KT: e.memset(KT[:, :, 0:128], 0.0), [], [("KT", 0)])
                R.op("pool", lambda e, V=V: e.memset(V[:, 0, :], 0.0), [], [("V", 0)])
            with ExitStack() as es:
                W = B.sb(es, "W_in", [128, 8, 4096], BF16)
                wsrc = win_d[l].rearrange("(kc p) n -> p kc n", p=128)
                for g in range(8):
                    B.dma("pool", W[:, :, g * 512:(g + 1) * 512], wsrc[:, :, g * 512:(g + 1) * 512],
                          ("W", g // 2), [], [("Win", g)])
                hbk = [B.sb(es, "a_hb%d" % i, [128, 8, 512], BF16) for i in range(2)]
                cs = [B.sb(es, "a_cs%d" % i, [128, 2, 512], F32) for i in range(1)] * 2
                uh = [B.sb(es, "a_uh%d" % i, [128, 512], F32) for i in range(2)]
                vb = [[B.sb(es, "a_vb%d_%d" % (cc, p), [128, 514], F32) for p in range(2)] for cc in range(4)]
                yy = [B.sb(es, "a_y%d" % i, [128, 512], F32) for i in range(2)]
                co = [B.sb(es, "a_co%d" % i, [128, 4, 512], BF16) for i in range(1)] * 2
                t1 = [B.sb(es, "a_t1_%d" % i, [128, 512], F32) for i in range(2)]
                t2 = [B.sb(es, "a_t2_%d" % i, [128, 512], F32) for i in range(2)]
                qc = [B.sb(es, "a_qc%d" % i, [128, 4, 512], BF16) for i in range(1)] * 2
                for cc in range(4):
                    R.op("pool", lambda e, cc=cc, vb=vb: e.memset(vb[cc][0][:, 0:2], 0.0), [], [("vbh", cc, 0)])
                rr = 0
                def load_hb(j):
                    t0, n, pp0 = blks[j]
                    s = j % 2
                    B.dma("sp", hbk[s][:, :, :n], Hb[l][:, :, t0:t0 + n], ("a_hb", s), hkeys("Hb", l, j), [("a_hb", s)])
                load_hb(0)
                for j, (t0, n, pp0) in enumerate(blks):
                    s = j % 2
                    if j + 1 < len(blks):
                        load_hb(j + 1)
                    if l == 0:
                        for _ in range(3):
                            if dense_cast_queue:
                                dense_cast_queue.pop(0)()
                    B.dma("sp", cs[s][:, 0, :n], cos_d[:, t0:t0 + n], ("a_cs", 0), [], [("a_cs", 0)])
                    B.dma("sp", cs[s][:, 1, :n], sin_d[:, t0:t0 + n], ("a_cs", 0), [], [("a_cs", 0)])
                    if l == 0 and j >= 2 and moe_cast_next[0] < 4 and not SPARSE:
                        issue_moe_cast(moe_cast_next[0])
                        moe_cast_next[0] += 1

                    def zmm(bk, g, c0, s=s, n=n):
                        B.mmg(ps[bk][:, :n], [(W[:, kc, g * 512 + c0:g * 512 + c0 + 128], hbk[s][:, kc, :n]) for kc in range(8)],
                              [("Win", g), ("a_hb", s)], [PK(bk)])
                    par = j % 2
                    for cc in range(4):
                        bb, bc, bh = B.bank(), B.bank(), B.bank()
                        zmm(bh, 2, cc * 128)
                        zmm(bc, 1, cc * 128)
                        zmm(bb, 0, cc * 128)
                        u = rr % 2
                        rr += 1
                        B.cp("act", uh[u][:, :n], ps[bh][:, :n], [PK(bh)], [("a_uh", u)])
                        B.tt("dve", vb[cc][par][:, 2:2 + n], ps[bc][:, :n], uh[u][:, :n], ALU.mult,
                             [PK(bc), ("a_uh", u)], [("vb", cc, par)])
                        B.ts("dve", yy[u][:, :n], vb[cc][par][:, 2:2 + n], convw[:, l, cc, 2:3], ALU.mult,
                             [("vb", cc, par), "convw"], [("a_y", u)])
                        B.stt(yy[u][:, :n], vb[cc][par][:, 1:1 + n], convw[:, l, cc, 1:2], yy[u][:, :n], ALU.mult, ALU.add,
                              [("vb", cc, par), ("vbh", cc, par), "convw", ("a_y", u)], [("a_y", u)])
                        B.stt(yy[u][:, :n], vb[cc][par][:, 0:n], convw[:, l, cc, 0:1], yy[u][:, :n], ALU.mult, ALU.add,
                              [("vb", cc, par), ("vbh", cc, par), "convw", ("a_y", u)], [("a_y", u)])
                        B.cp("pool", vb[cc][1 - par][:, 0:2], vb[cc][par][:, n:n + 2],
                             [("vb", cc, par), ("vbh", cc, par)], [("vbh", cc, 1 - par)])
                        B.tt("dve", co[s][:, cc, :n], ps[bb][:, :n], yy[u][:, :n], ALU.mult,
                             [PK(bb), ("a_y", u)], [("a_co", 0, cc)])
                    B.dma("sp", CO[l][:, :, t0:t0 + n], co[s][:, :, :n], "a_cos",
                          [("a_co", 0, cc) for cc in range(4)], [("CO", l, j)])
                    for which in range(2):
                        for hh in range(4):
                            b1, b2 = B.bank(), B.bank()
                            zmm(b1, 3 + which, hh * 128)
                            zmm(b2, 6 + which, hh * 128)
                            u = rr % 2
                            rr += 1
                            B.tt("dve", t1[u][:, :n], ps[b1][:, :n], cs[s][:, 0, :n], ALU.mult,
                                 [PK(b1), ("a_cs", 0)], [("a_t1", u)])
                            B.tt("dve", t2[u][:, :n], ps[b2][:, :n], cs[s][:, 1, :n], ALU.mult,
                                 [PK(b2), ("a_cs", 0)], [("a_t2", u)])
                            if which == 0:
                                B.tt("pool", qc[s][:, hh, :n], t1[u][:, :n], t2[u][:, :n], ALU.add,
                                     [("a_t1", u), ("a_t2", u)], [("a_qc", 0, hh)])
                            else:
                                B.tt("pool", KT[:, hh, pp0:pp0 + n], t1[u][:, :n], t2[u][:, :n], ALU.add,
                                     [("a_t1", u), ("a_t2", u)], [("KT", j, hh)] if j else [("KT", 0), ("KT", j, hh)])
                    B.dma("sp", QS[l][:, :, t0:t0 + n], qc[s][:, :, :n], "a_qs",
                          [("a_qc", 0, hh) for hh in range(4)], [("QS", l, j)])
                    nsub = 1 if j == 0 else 4
                    for sub in range(nsub):
                        r = NMETA if j == 0 else 128
                        ti = 0 if j == 0 else 4 * (j - 1) + 1 + sub
                        bk = B.bank()
                        B.mmg(ps[bk][:r, :], [(hbk[s][:, kc, sub * 128:sub * 128 + r], W[:, kc, 5 * 512:6 * 512]) for kc in range(8)],
                              [("Win", 5), ("a_hb", s)], [PK(bk)])
                        B.cp("act", V[:r, ti, :], ps[bk][:r, :], [PK(bk)], [("V", ti)])
            while l == 0 and dense_cast_queue:
                dense_cast_queue.pop(0)()
            R.barrier()
            if stop_after == ("A", l):
                break
            with ExitStack() as es:
                Wo = B.sb(es, "W_out", [128, 8, D], BF16)
                wsrc = wout_d[l].rearrange("(kc p) n -> p kc n", p=128)
                for g in range(2):
                    B.dma("pool", Wo[:, :, g * 512:(g + 1) * 512], wsrc[:, :, g * 512:(g + 1) * 512],
                          "Wo", [], [("Wout", g)])
                if l == 0:
                    while moe_cast_next[0] < 4:
                        issue_moe_cast(moe_cast_next[0])
                        moe_cast_next[0] += 1
                Qa = [B.sb(es, "b_qa%d" % i, [128, 4, 512], BF16) for i in range(2)]
                Qb = [B.sb(es, "b_qb%d" % i, [128, 4, 512], BF16) for i in range(2)]
                cat = [B.sb(es, "b_cat%d" % i, [128, 8, 512], BF16) for i in range(1)] * 2
                hfk = [B.sb(es, "b_hf%d" % i, [128, 8, 512], F32) for i in range(1)] * 2
                NPT = 4
                pt = [B.sb(es, "b_pt%d" % i, [128, 512], BF16) for i in range(NPT)]
                r12 = B.sb(es, "b_r12", [128, 2, 512], F32)
                oab = B.sb(es, "b_oab", [128, 2, 512], F32)
                ot = B.sb(es, "b_o", [128, 512], F32)
                osq = B.sb(es, "b_osq", [128, 512], BF16)
                rs = B.sb(es, "b_rs", [128, 512], F32)
                x = B.sb(es, "b_x", [128, 8, 512], F32)
                xbs = B.sb(es, "b_xbs", [128, 2, 512], BF16)
                xsqs = B.sb(es, "b_xsqs", [128, 2, 512], BF16)
                tmp = B.sb(es, "b_tmp", [128, 2, 512], F32)
                small = B.sb(es, "b_small", [128, 4, 512], F32)
                sp_last = SPARSE and last
                ob = [None if sp_last else B.sb(es, "b_ob%d" % i, [128, 8, 512], BF16) for i in range(1)]
                if sp_last:
                    mtf = [B.sb(es, "b_mtf%d" % i, [128, D], F32) for i in range(2)]
                    mtb = [B.sb(es, "b_mtb%d" % i, [128, D], BF16) for i in range(2)]
                mtcs = [0]
                pending_post = []
                zero_queue = []
                if SPARSE and l == 0:
                    zt = B.sb(es, "b_zero", [128, 4096], BF16)
                    R.op("pool", lambda e, zt=zt: e.memset(zt[:], 0.0), [], ["b_zero"])
                    for w_ in range(NITEM):
                        zero_queue.append(lambda w_=w_, zt=zt: B.dma(
                            "sp", Xg[w_ * 512:(w_ + 1) * 512, :].rearrange("(p a) d -> p (a d)", a=4), zt[:], "b_z",
                            ["b_zero"], [("Xgz", w_)]))
                for i in range(2):
                    R.op("pool", lambda e, i=i, Qa=Qa: e.memset(Qa[i][64:128, :, :], 0.0), [], [("b_qaz", i)])
                    R.op("pool", lambda e, i=i, Qb=Qb: e.memset(Qb[i][0:64, :, :], 0.0), [], [("b_qbz", i)])
                ACC = (0, 1, 2, 3)
                STP = (4, 5, 6, 7)
                ptc = 0
                def load_q(j):
                    t0, n, pp0 = blks[j]
                    s = j % 2
                    B.dma("sp", Qa[s][0:64, :, :n], QS[l][0:64, :, t0:t0 + n], ("b_q", s), [("QS", l, j)], [("b_qa", s)])
                    B.dma("sp", Qb[s][64:128, :, :n], QS[l][64:128, :, t0:t0 + n], ("b_q", s), [("QS", l, j)], [("b_qb", s)])
                load_q(1 if last else 0)
                for j, (t0, n, pp0) in enumerate(blks):
                    if last and j == 0:
                        continue
                    s = j % 2
                    if j + 1 < len(blks):
                        load_q(j + 1)
                    B.dma("sp", cat[s][:, 0:4, :n], CO[l][:, :, t0:t0 + n], "b_co", [("CO", l, j)],
                          [("b_cat", 0, c) for c in range(4)])
                    B.dma("sp", hfk[s][:, :, :n], Hf[l][:, :, t0:t0 + n], ("b_hf", 0), hkeys("Hf", l, j), [("b_hf", 0)])
                    tiles = [0] if j == 0 else [0] + list(range(1, 4 * j + 1))
                    diag = {0: 0} if j == 0 else {4 * (j - 1) + 1 + r: r for r in range(4)}

                    def tile_blk(i):
                        return 0 if i == 0 else (i - 1) // 4 + 1
                    units = [(hh, m, i) for hh in range(4) for m in range(2) for i in tiles]
                    LA = 3
                    info = {}
                    for idx in range(len(units) + LA):
                        if idx < len(units):
                            hh, m, i = units[idx]
                            if m == 0 and i == tiles[0] and l == 0 and j >= 1:
                                pump_casts(5)
                                if zero_queue:
                                    zero_queue.pop(0)()
                            bk = B.bank(STP)
                            Qm = Qa if m == 0 else Qb
                            qk = [("b_qa", s), ("b_qaz", s)] if m == 0 else [("b_qb", s), ("b_qbz", s)]
                            isd = i in diag
                            kk = [("KT", tile_blk(i), hh)] + ([("KT", 0)] if i == 0 else [])
                            B.mm(ps[bk][:, :n], KT[:, hh, i * 128:(i + 1) * 128], Qm[s][:, hh, :n], True, not isd,
                                 kk + qk, [PK(bk)])
                            if isd:
                                B.mm(ps[bk][:, :n], identb, maskb[:, diag[i], :n], False, True,
                                     ["cb", "maskb"], [PK(bk)])
                            p = ptc % NPT
                            ptc += 1
                            B.act(pt[p][:, :n], ps[bk][:, :n], AF.Exp, [PK(bk)], [("b_pt", p)], scale=0.125)
                            info[idx] = p
                        if idx >= LA:
                            hh, m, i = units[idx - LA]
                            p = info[idx - LA]
                            first = (i == tiles[0])
                            lastt = (i == tiles[-1])
                            sset = (2 * hh + m) % 2
                            bo, bs = ACC[2 * sset], ACC[2 * sset + 1]
                            B.mm(ps[bo][:, :n], V[:, i, hh * 128:(hh + 1) * 128], pt[p][:, :n], first, lastt,
                                 [("V", i), ("b_pt", p)] + ([("V", 0)] if i == 0 else []), [PK(bo)])
                            B.mm(ps[bs][:, :n], ones0b if i == 0 else onesb, pt[p][:, :n], first, lastt,
                                 ["cb", ("b_pt", p)], [PK(bs)])
                            if lastt:
                                B.cp("act", r12[:, m, :n], ps[bs][:, :n], [PK(bs)], [("b_r", m)])
                                B.cp("act", oab[:, m, :n], ps[bo][:, :n], [PK(bo)], [("b_oab", m)])
                                if m == 1:
                                    for m in range(2):
                                        R.op("dve", lambda e, m=m, n=n, r12=r12: e.reciprocal(out=r12[:, m, :n], in_=r12[:, m, :n]),
                                             [("b_r", m)], [("b_r", m)])
                                        B.tt("dve", oab[:, m, :n], oab[:, m, :n], r12[:, m, :n], ALU.mult,
                                             [("b_oab", m), ("b_r", m)], [("b_oab", m)])
                                    B.stt(ot[:, :n], oab[:, 1, :n], neglam[:, l, :], oab[:, 0, :n], ALU.mult, ALU.add,
                                          [("b_oab", 0), ("b_oab", 1), ("neglam", l)], ["b_o"])
                                    B.act(osq[:, :n], ot[:, :n], AF.Square, ["b_o"], ["b_osq"])
                                    bk = B.bank(STP)
                                    B.mm(ps[bk][:, :n], ones128, osq[:, :n], True, True, ["cb", "b_osq"], [PK(bk)])
                                    B.act(rs[:, :n], ps[bk][:, :n], AF.Ln, [PK(bk), "epst"], ["b_rs0"], bias=epst[:, 0:1])
                                    B.act(rs[:, :n], rs[:, :n], AF.Exp, ["b_rs0"], ["b_rs"], scale=-0.5)
                                    B.stt(cat[s][:, 4 + hh, :n], ot[:, :n], gsc[:, l, :], rs[:, :n], ALU.mult, ALU.mult,
                                          ["b_o", "b_rs", ("gsc", l)], [("b_cat", 0, 4 + hh)])
                                    if hh == 0:
                                        while pending_post:
                                            pending_post.pop(0)()
                    bm, bq = ACC[0], ACC[1]
                    for dc in range(8):
                        bk = B.bank(STP)
                        B.mmg(ps[bk][:, :n], [(Wo[:, kc, dc * 128:(dc + 1) * 128], cat[s][:, kc, :n]) for kc in range(8)],
                              [("Wout", dc // 4)] + [("b_cat", 0, c) for c in range(8)], [PK(bk)])
                        B.stt(x[:, dc, :n], hfk[s][:, dc, :n], ALPHA, ps[bk][:, :n], ALU.mult, ALU.add,
                              [("b_hf", 0), PK(bk)], [("x", dc)])
                        ln_stats_dc(bm, bq, dc, n, x, xbs, xsqs)
                    ln_finish(bm, bq, n, x, tmp, small,
                              lambda dc: lngb[:, l, 0, dc:dc + 1], lambda dc: lngb[:, l, 1, dc:dc + 1],
                              ob[0], "b_ob")
                    def post_block(j=j, t0=t0, n=n):
                        if not sp_last:
                            B.dma("sp", Mf[l][:, :, t0:t0 + n], x[:, :, :n], "b_sf", [("x", dc) for dc in range(8)], [("Mf", l, j)])
                            B.dma("sp", Mb[l][:, :, t0:t0 + n], ob[0][:, :, :n], "b_sf", [("b_ob", dc) for dc in range(8)], [("Mb", l, j)])
                        else:
                            for q in range(4):
                                tt = 4 * (j - 1) + q
                                xk = [("x", dc) for dc in range(8)]
                                import os as _os
                                if not _os.environ.get("NO_ROUTER"):
                                    br = B.bank(STP)
                                    B.mmg(ps[br][:, 0:NE], [(x[:, kc, q * 128:(q + 1) * 128], wr[:, kc, :]) for kc in range(8)],
                                          xk + ["wr"], [PK(br)])
                                    B.cp("dve", LG[:, tt, :], ps[br][:, 0:NE], [PK(br)], [("LG", tt)])
                                if _os.environ.get("NO_TR"):
                                    continue
                                ms = mtcs[0] % 2
                                mtcs[0] += 1
                                for half in range(2):
                                    bk = B.bank(STP)

                                    def trb(e, q=q, half=half, bk=bk):
                                        ins = None
                                        for qq in range(4):
                                            dc = half * 4 + qq
                                            ins = e.transpose(ps[bk][:, qq * 128:(qq + 1) * 128],
                                                              x[:, dc, q * 128:(q + 1) * 128], identf[:, :])
                                        return ins
                                    R.op("pe", trb, xk + ["identf"], [PK(bk)])
                                    B.cp("act", mtf[ms][:, half * 512:(half + 1) * 512], ps[bk][:, :], [PK(bk)], [("b_mtf", ms, half)])
                                    B.cp("dve", mtb[ms][:, half * 512:(half + 1) * 512], ps[bk][:, :], [PK(bk)], [("b_mtb", ms, half)])
                                B.dma("sp", MTf[tt * 128:(tt + 1) * 128, :], mtf[ms][:], ("b_smf", ms),
                                      [("b_mtf", ms, 0), ("b_mtf", ms, 1)], [("MTf", tt)])
                                B.dma("sp", MTb[tt * 128:(tt + 1) * 128, :], mtb[ms][:], ("b_smb", ms),
                                      [("b_mtb", ms, 0), ("b_mtb", ms, 1)], [("MTb", tt)])
                    pending_post.append(post_block)
                while pending_post:
                    pending_post.pop(0)()
                while zero_queue:
                    zero_queue.pop(0)()
            R.barrier()
            if stop_after == ("B", l):
                break
        if SPARSE and moe:
            sparse_moe_phase(l)
            R.barrier()
            continue
        with ExitStack() as es:
            F = FE if moe else FD
            nexp = NE if moe else 1
            if moe and dbg_nexp is not None:
                nexp = dbg_nexp
            FC = F // 128
            GC = 512 if moe else 256
            NG = F // GC
            CPG = GC // 128
            DG = 4 if moe else 2
            NDG = FC // DG
            lw = 1 if moe else 0
            NWB = 2
            assert not moe, "dense-all-experts MoE path retired: use SPARSE"
            NWB = 3
            wgu = [B.sb(es, "c_wgu%d" % i, [128, 2, 8, GC], BF16) for i in range(NWB)]
            wdt = [B.sb(es, "c_wd%d" % i, [128, DG, D], BF16) for i in range(NWB)]
            NHB = 1 if moe else 2
            hmb = [B.sb(es, "c_hmb%d" % i, [128, 8, 512], BF16) for i in range(NHB)]
            hmf = [B.sb(es, "c_hmf%d" % i, [128, 8, 512], F32) for i in range(NHB)]
            actt = B.sb(es, "c_act", [128, FC, 512], BF16)
            sg = [B.sb(es, "c_sg%d" % i, [128, 512], F32) for i in range(3)]
            x = B.sb(es, "c_x", [128, 8, 512], F32)
            xbs = B.sb(es, "c_xbs", [128, 2, 512], BF16)
            xsqs = B.sb(es, "c_xsqs", [128, 2, 512], BF16)
            tmp = B.sb(es, "c_tmp", [128, 2, 512], F32)
            small = B.sb(es, "c_small", [128, 4, 512], F32)
            of = x
            ob = None if last else B.sb(es, "c_ob", [128, 8, 512], BF16)
            if moe:
                acc = x
                G = B.sb(es, "c_G", [128, NE, 512], F32)
                lts = B.sb(es, "c_lts", [8, 512], F32)
                m12 = B.sb(es, "c_m12", [128, 4, 512], F32)
            if last:
                yt = [B.sb(es, "c_yt%d" % i, [128, D], F32) for i in range(2)]
            wc = 0
            dwc = 0
            sgc = 0
            ytc = 0
            pending_c = []
            for j, (t0, n, pp0) in enumerate(blks):
                if last and j == 0:
                    continue
                if moe and dbg_nblk is not None and j > dbg_nblk:
                    continue
                s = j % NHB

                def load_hm(j):
                    t0, n, pp0 = blks[j]
                    s = j % NHB
                    B.dma("sp", hmb[s][:, :, :n], Mb[l][:, :, t0:t0 + n], ("c_hmb", s), [("Mb", l, j)], [("c_hmb", s)])
                    B.dma("sp", hmf[s][:, :, :n], Mf[l][:, :, t0:t0 + n], ("c_hmf", s), [("Mf", l, j)], [("c_hmf", s)])
                if j == 0:
                    load_hm(0)
                if j + 1 < len(blks):
                    load_hm(j + 1)
                if moe:
                    bk = B.bank()
                    B.mmg(ps[bk][:8, :n], [(wr[:, kc, :], hmf[s][:, kc, :n]) for kc in range(8)],
                          ["wr", ("c_hmf", s)], [PK(bk)])
                    B.cp("act", lts[:, :n], ps[bk][:8, :n], [PK(bk)], ["c_lts"])
                    for e_ in range(NE):
                        bk = B.bank()
                        B.mm(ps[bk][:, :n], sel[:, e_, :], lts[:, :n], True, True, ["sel", "c_lts"], [PK(bk)])
                        B.cp("act", G[:, e_, :n], ps[bk][:, :n], [PK(bk)], [("c_G", e_)])
                    m1, m2, tq, dd = (m12[:, i, :n] for i in range(4))
                    B.tt("dve", m1, G[:, 0, :n], G[:, 1, :n], ALU.max, [("c_G", 0), ("c_G", 1)], ["c_m1"])
                    B.tt("dve", m2, G[:, 0, :n], G[:, 1, :n], ALU.min, [("c_G", 0), ("c_G", 1)], ["c_m2"])
                    for e_ in range(2, NE):
                        B.tt("dve", tq, m1, G[:, e_, :n], ALU.min, ["c_m1", ("c_G", e_)], ["c_tq"])
                        B.tt("dve", m2, m2, tq, ALU.max, ["c_m2", "c_tq"], ["c_m2"])
                        B.tt("dve", m1, m1, G[:, e_, :n], ALU.max, ["c_m1", ("c_G", e_)], ["c_m1"])
                    B.tt("dve", dd, m2, m1, ALU.subtract, ["c_m1", "c_m2"], ["c_dd"])
                    B.act(dd, dd, AF.Exp, ["c_dd"], ["c_dd"])
                    B.ts("dve", dd, dd, 1.0, ALU.add, ["c_dd"], ["c_dd"])
                    R.op("dve", lambda e, dd=dd: e.reciprocal(out=dd, in_=dd), ["c_dd"], ["c_dd"])
                    for e_ in range(NE):
                        B.tt("dve", tq, G[:, e_, :n], m2, ALU.is_ge, [("c_G", e_), "c_m2"], ["c_tq"])
                        B.tt("dve", G[:, e_, :n], G[:, e_, :n], m1, ALU.subtract, [("c_G", e_), "c_m1"], [("c_G", e_)])
                        B.act(G[:, e_, :n], G[:, e_, :n], AF.Exp, [("c_G", e_)], [("c_G", e_)])
                        B.tt("dve", G[:, e_, :n], G[:, e_, :n], tq, ALU.mult, [("c_G", e_), "c_tq"], [("c_G", e_)])
                        B.tt("dve", G[:, e_, :n], G[:, e_, :n], dd, ALU.mult, [("c_G", e_), "c_dd"], [("c_G", e_)])
                for e_ in range(nexp):
                    gname, uname, dname = ("g1_%d" % (e_ // 2), "u1_%d" % (e_ // 2), "d1_%d" % (e_ // 2)) if moe else ("g0", "u0", "d0")
                    for g in range(NG):
                        if l == 0:
                            pump_casts(1)
                        if g == 2:
                            while pending_c:
                                pending_c.pop(0)()
                        w = wc % NWB
                        wc += 1
                        B.dma("sp", wgu[w][:, 0].rearrange("p k c -> p (k c)"), WGd2[g * 128:(g + 1) * 128, :],
                              ("c_wgu", w), dense_cast_keys, [("c_wgu", w)])
                        B.dma("sp", wgu[w][:, 1].rearrange("p k c -> p (k c)"), WUd2[g * 128:(g + 1) * 128, :],
                              ("c_wgu", w), dense_cast_keys, [("c_wgu", w)])
                        for c in range(CPG):
                            fc = g * CPG + c
                            bg, bu = B.bank(), B.bank()
                            B.mmg(ps[bg][:, :n], [(wgu[w][:, 0, kc, c * 128:(c + 1) * 128], hmb[s][:, kc, :n]) for kc in range(8)],
                                  [("c_wgu", w), ("c_hmb", s)], [PK(bg)])
                            B.mmg(ps[bu][:, :n], [(wgu[w][:, 1, kc, c * 128:(c + 1) * 128], hmb[s][:, kc, :n]) for kc in range(8)],
                                  [("c_wgu", w), ("c_hmb", s)], [PK(bu)])
                            q = sgc % 3
                            sgc += 1
                            B.act(sg[q][:, :n], ps[bg][:, :n], AF.Silu, [PK(bg)], [("c_sg", q)])
                            B.tt("dve", actt[:, fc, :n], ps[bu][:, :n], sg[q][:, :n], ALU.mult,
                                 [PK(bu), ("c_sg", q)], [("c_act", fc)])
                    for dg in range(NDG):
                        w = dwc % NWB
                        dwc += 1
                        B.dma("sp", wdt[w][:].rearrange("p c d -> p (c d)"), WDd2[dg * 128:(dg + 1) * 128, :],
                              ("c_wd", w), dense_cast_keys, [("c_wd", w)])
                        for dc in range(8):
                            for c in range(DG):
                                fc = dg * DG + c
                                B.mm(ps[dc][:, :n], wdt[w][:, c, dc * 128:(dc + 1) * 128], actt[:, fc, :n],
                                     fc == 0, fc == FC - 1, [("c_wd", w), ("c_act", fc)], [PK(dc)])
                    for dc in range(8):
                        if not moe:
                            B.stt(x[:, dc, :n], hmf[s][:, dc, :n], ALPHA, ps[dc][:, :n], ALU.mult, ALU.add,
                                  [("c_hmf", s), PK(dc)], [("x", dc)])
                        elif e_ == 0:
                            B.tt("dve", acc[:, dc, :n], ps[dc][:, :n], G[:, 0, :n], ALU.mult,
                                 [PK(dc), ("c_G", 0)], [("x", dc)])
                        else:
                            t = tmp[:, dc % 2, :n]
                            tk = ("lnt", dc % 2)
                            B.tt("dve", t, ps[dc][:, :n], G[:, e_, :n], ALU.mult, [PK(dc), ("c_G", e_)], [tk])
                            B.tt("pool", acc[:, dc, :n], acc[:, dc, :n], t, ALU.add, [("x", dc), tk], [("x", dc)])
                B.bank_rr = 0
                for dc in range(8):
                    if moe:
                        B.stt(x[:, dc, :n], hmf[s][:, dc, :n], ALPHA, x[:, dc, :n], ALU.mult, ALU.add,
                              [("c_hmf", s), ("x", dc)], [("x", dc)])
                def c_tail(j=j, t0=t0, n=n):
                    bm, bq = B.bank(), B.bank()
                    for dc in range(8):
                        ln_stats_dc(bm, bq, dc, n, x, xbs, xsqs)
                    ln_finish(bm, bq, n, x, tmp, small,
                              lambda dc: lngb[:, l, 2, dc:dc + 1], lambda dc: lngb[:, l, 3, dc:dc + 1],
                              ob, "c_ob")
                    B.dma("sp", Hf[l + 1][:, :, t0:t0 + n], of[:, :, :n], "c_sf", [("x", dc) for dc in range(8)], [("Hf", l + 1, j)])
                    B.dma("sp", Hb[l + 1][:, :, t0:t0 + n], ob[:, :, :n], "c_sf", [("c_ob", dc) for dc in range(8)], [("Hb", l + 1, j)])
                if not last:
                    pending_c.append(c_tail)
                else:
                    raise AssertionError("dense path is only used for non-final layers")
                    for sub in range(4):
                        q = ytc % 2
                        ytc += 1
                        for half in range(2):
                            bk = B.bank()

                            def tr(e, sub=sub, half=half, bk=bk):
                                ins = None
                                for qq in range(4):
                                    dc = half * 4 + qq
                                    ins = e.transpose(ps[bk][:, qq * 128:(qq + 1) * 128],
                                                      of[:, dc, sub * 128:(sub + 1) * 128], identf[:, :])
                                return ins
                            R.op("pe", tr, [("x", half * 4 + qq) for qq in range(4)] + ["identf"], [PK(bk)])
                            B.cp("act" if half == 0 else "dve", yt[q][:, half * 512:(half + 1) * 512], ps[bk][:, :],
                                 [PK(bk)], [("c_yt", q, half)])
                        row0 = t0 - NMETA + sub * 128
                        final_ops.append(B.dma("sp", y_d[row0:row0 + 128, :], yt[q][:], ("c_y", q),
                                               [("c_yt", q, 0), ("c_yt", q, 1)], []))
            while pending_c:
                pending_c.pop(0)()
        pump_casts(len(cast_queue))
        R.barrier()
        if stop_after == ("C", l):
            break

    R.emit(nc, final_wait_ops=sorted({op.sem for op in final_ops}))
    return B


def _consts():
    inv = (1.0 / (np.float32(10000.0) ** (np.arange(0, 64, 2, dtype=np.float32) / np.float32(64)))).astype(np.float32)
    ang = np.arange(L, dtype=np.float32)[:, None] * inv[None, :]
    ang = np.concatenate([ang, ang], axis=-1).astype(np.float32)
    cos = np.cos(ang).astype(np.float32).T
    sin = np.sin(ang).astype(np.float32).T
    sgn = np.where(np.arange(64) < 32, -1.0, 1.0).astype(np.float32)[:, None]
    cosT = np.concatenate([cos, cos], axis=0)
    sinT = np.concatenate([sin * sgn, sin * sgn], axis=0)
    p = np.arange(128)[:, None, None]
    r = np.arange(4)[None, :, None]
    f = np.arange(512)[None, None, :]
    maskb = np.where(f >= 128 * r + p, 0.0, NEG).astype(ml_dtypes.bfloat16)
    identf = np.eye(128, dtype=np.float32)
    cb = np.zeros((128, 5, 128), np.float32)
    cb[:, 0, :] = np.eye(128)
    cb[:, 1, :] = 1.0
    cb[:16, 2, :] = 1.0
    cb[:, 3, :] = 1.0 / 1024.0
    cb[:, 4, :] = 1.0 / 128.0
    sel = np.zeros((8, NE, 128), np.float32)
    for e in range(NE):
        sel[e, e, :] = 1.0
    pconst = (np.arange(7)[None, :] * 128 + np.arange(128)[:, None]).astype(np.float32)
    constf = np.zeros((128, 2, 128), np.float32)
    constf[:, 0, :] = 1.0
    constf[:, 1, :] = (np.arange(128)[:, None] < np.arange(128)[None, :]).astype(np.float32)
    return dict(cosT=np.ascontiguousarray(cosT), sinT=np.ascontiguousarray(sinT), maskb=maskb,
                identf=identf, constb=cb.astype(ml_dtypes.bfloat16), sel=sel, constf=constf, pconst=pconst)


def _fm(v):
    return np.ascontiguousarray(np.asarray(v, np.float32).reshape(8, 128).T)


def _prep_shared(inp):
    w_in = np.asarray(inp["w_in"], np.float32)
    perm = np.concatenate([np.arange(32, 64), np.arange(0, 32)])
    qcols = w_in[:, :, 1536:2048].reshape(DEPTH, D, 8, 64)[:, :, :, perm].reshape(DEPTH, D, 512)
    kcols = w_in[:, :, 2048:2560].reshape(DEPTH, D, 8, 64)[:, :, :, perm].reshape(DEPTH, D, 512)
    w_in_ext = np.ascontiguousarray(np.concatenate([w_in, qcols, kcols], axis=2))
    conv_w = np.asarray(inp["conv_w"], np.float32)
    convw = np.ascontiguousarray(conv_w.reshape(DEPTH, 3, 4, 128).transpose(0, 3, 2, 1))
    lam = np.stack([np.asarray(inp[k], np.float32) for k in ("lambda_q1", "lambda_k1", "lambda_q2", "lambda_k2")], axis=1)
    lam = np.ascontiguousarray(np.broadcast_to(lam[:, None], (DEPTH, 128, 4, 64)))
    subg = np.ascontiguousarray(np.asarray(inp["subln_g"], np.float32).reshape(DEPTH, 128, 1))
    lngb = np.stack([np.stack([_fm(inp[k][l]) for k in ("ln_mix_g", "ln_mix_b", "ln_ffn_g", "ln_ffn_b")], axis=1)
                     for l in range(DEPTH)], axis=0)
    embgb = np.stack([_fm(inp["ln_emb_g"]), _fm(inp["ln_emb_b"])], axis=1)
    wr = np.ascontiguousarray(np.asarray(inp["w_router"], np.float32)[0].reshape(8, 128, NE).transpose(1, 0, 2))
    gbrep = np.ascontiguousarray(np.broadcast_to(
        np.stack([np.asarray(inp["ln_ffn_g"], np.float32)[DEPTH - 1], np.asarray(inp["ln_ffn_b"], np.float32)[DEPTH - 1]], axis=0)[None],
        (128, 2, D)))
    sh = dict(
        gbrep=gbrep, meta=np.ascontiguousarray(np.asarray(inp["meta_tokens"], np.float32)),
        embgb=np.ascontiguousarray(embgb), w_in_ext=w_in_ext, convw=convw, lam=lam, subg=subg,
        w_out=np.ascontiguousarray(np.asarray(inp["w_out"], np.float32)), lngb=np.ascontiguousarray(lngb),
        w_gate_dense=np.asarray(inp["w_gate_dense"], np.float32), w_up_dense=np.asarray(inp["w_up_dense"], np.float32),
        w_down_dense=np.asarray(inp["w_down_dense"], np.float32), w_router=wr,
        w_gate_moe=np.asarray(inp["w_gate_moe"], np.float32), w_up_moe=np.asarray(inp["w_up_moe"], np.float32),
        w_down_moe=np.asarray(inp["w_down_moe"], np.float32),
    )
    sh.update(_consts())
    return sh


def kernel(**inputs):
    x = np.asarray(inputs["x"], np.float32)
    sh = _prep_shared(inputs)
    B = build_program(debug=False)
    in_maps = []
    for c in range(8):
        m = dict(sh)
        m["x"] = np.ascontiguousarray(x[c])
        in_maps.append(m)
    res = run_bass_kernel_spmd(B.nc, in_maps, core_ids=list(range(8)))
    return np.stack([np.asarray(r["y"], np.float32) for r in res.results], axis=0)
```

```python
import math
from contextlib import ExitStack

import numpy as np
import ml_dtypes

import concourse.bass as bass
import concourse.mybir as mybir
from concourse.bass_utils import run_bass_kernel_spmd

F32 = mybir.dt.float32
BF16 = mybir.dt.bfloat16
ALU = mybir.AluOpType
AF = mybir.ActivationFunctionType

D = 1024
SEQ = 4096
NMETA = 16
L = SEQ + NMETA
LP = 128 + SEQ
DEPTH = 2
FD = 2816
FE = 3584
NE = 8
ALPHA = (2 * DEPTH) ** 0.25
LN_EPS = 1e-5
NEG = -30000.0

ENGS = ("pe", "act", "dve", "pool", "sp")
I32 = mybir.dt.int32
SPARSE = True
NITEM = 23

DEBUG = False


class Op:
    __slots__ = ("eng", "fn", "deps", "is_dma", "sem", "val", "need_inc")

    def __init__(self, eng, fn, is_dma, sem):
        self.eng = eng
        self.fn = fn
        self.deps = []
        self.is_dma = is_dma
        self.sem = sem
        self.val = 0
        self.need_inc = is_dma


class Rec:
    def __init__(self):
        self.ops = {e: [] for e in ENGS}
        self.last_w = {}
        self.rd_eng = {}
        self.rd_dma = {}
        self.dma_cnt = {}
        self.pending = {}
        self.dmas_since_barrier = []

    def barrier(self):
        deps = []
        for e in ENGS:
            last = None
            for op in reversed(self.ops[e]):
                if not op.is_dma:
                    last = op
                    break
            if last is not None:
                deps.append(last)
        deps.extend(self.dmas_since_barrier)
        self.dmas_since_barrier = []
        for e in ENGS:
            self.pending[e] = list(deps)

    def _add(self, eng, fn, reads, writes, is_dma=False, sem=None):
        op = Op(eng, fn, is_dma, sem)
        psr = [k for k in reads if isinstance(k, tuple) and k and k[0] == "ps"]
        if psr:
            writes = list(writes) + [k for k in psr if k not in writes]
        deps = {}
        raw = set()
        for k in reads:
            w = self.last_w.get(k)
            if w is not None:
                deps[id(w)] = w
                raw.add(id(w))
        for k in writes:
            w = self.last_w.get(k)
            if w is not None:
                deps[id(w)] = w
            for r in self.rd_eng.get(k, {}).values():
                deps[id(r)] = r
            for r in self.rd_dma.get(k, ()):
                deps[id(r)] = r
        for d in self.pending.pop(eng, ()):
            deps[id(d)] = d
            raw.add(id(d))
        for d in deps.values():
            if d is op:
                continue
            if (not is_dma) and (not d.is_dma) and d.eng == eng:
                if eng == "pe":
                    continue
            op.deps.append((d, 16 * self.dma_cnt[d.sem] if d.is_dma else None))
            d.need_inc = True
        for k in reads:
            if is_dma:
                self.rd_dma.setdefault(k, []).append(op)
            else:
                self.rd_eng.setdefault(k, {})[eng] = op
        for k in writes:
            self.last_w[k] = op
            self.rd_eng[k] = {}
            self.rd_dma[k] = []
        if is_dma:
            self.dma_cnt[sem] = self.dma_cnt.get(sem, 0) + 1
            self.dmas_since_barrier.append(op)
        self.ops[eng].append(op)
        return op

    def op(self, eng, fn, reads=(), writes=()):
        return self._add(eng, fn, reads, writes)

    def dma(self, eng, fn, sem, reads=(), writes=(), background=False):
        self.dma_cnt.setdefault(sem, 0)
        op = self._add(eng, fn, reads, writes, is_dma=True, sem=sem)
        if background:
            self.dmas_since_barrier.pop()
        return op

    def emit(self, nc, final_wait_ops=()):
        cnt = {e: 0 for e in ENGS}
        for e in ENGS:
            for op in self.ops[e]:
                if (not op.is_dma) and op.need_inc:
                    cnt[e] += 1
                    op.val = cnt[e]
        with ExitStack() as st:
            esem = {e: st.enter_context(nc.semaphore("s_" + e)) for e in ENGS}
            dsem = {k: st.enter_context(nc.semaphore("d_%d" % i))
                    for i, k in enumerate(self.dma_cnt)}
            block = st.enter_context(nc.Block())
            beng = {"pe": block.tensor, "act": block.scalar, "dve": block.vector,
                    "pool": block.gpsimd, "sp": block.sync}

            def mk(e):
                def body(eng):
                    waited = {}
                    for op in self.ops[e]:
                        for d, dv in op.deps:
                            key = ("d", d.sem) if d.is_dma else ("e", d.eng)
                            v = dv if d.is_dma else d.val
                            if waited.get(key, 0) >= v:
                                continue
                            waited[key] = v
                            eng.wait_ge(dsem[d.sem] if d.is_dma else esem[d.eng], v)
                        ins = op.fn(eng)
                        if op.is_dma:
                            ins.then_inc(dsem[op.sem], 16)
                        elif op.need_inc:
                            ins.then_inc(esem[e], 1)
                    if e == "sp":
                        for sem_key in final_wait_ops:
                            eng.wait_ge(dsem[sem_key], 16 * self.dma_cnt[sem_key])
                return body

            for e in ENGS:
                beng[e](mk(e))
        return cnt


def token_blocks():
    blks = [(0, NMETA, 0)]
    for j in range(1, 9):
        blks.append((NMETA + 512 * (j - 1), 512, 128 + 512 * (j - 1)))
    return blks


class Builder:
    def __init__(self, debug=False):
        self.debug = debug
        self.nc = bass.Bass("TRN2", target_bir_lowering=False)
        self.R = Rec()
        self.st = ExitStack()
        self.bank_rr = 0
        self.uid = 0
        self.sbuf_bytes = 0

    def din(self, name, shape, dt=F32):
        return self.nc.dram_tensor(name, list(shape), dt, kind="ExternalInput").ap()

    def dout(self, name, shape, dt=F32):
        return self.nc.dram_tensor(name, list(shape), dt, kind="ExternalOutput").ap()

    def dscr(self, name, shape, dt):
        if self.debug:
            return self.nc.dram_tensor(name, list(shape), dt, kind="ExternalOutput").ap()
        return self.nc.dram_tensor(name, list(shape), dt).ap()

    def sb(self, stack, name, shape, dt):
        n = 1
        for s in shape[1:]:
            n *= s
        self.sbuf_bytes += n * (4 if dt == F32 else 2)
        self.uid += 1
        return stack.enter_context(self.nc.sbuf_tensor("sb%d_%s" % (self.uid, name), list(shape), dt))

    def bank(self, pool=None):
        pool = pool or (0, 1, 2, 3, 4, 5, 6, 7)
        b = pool[self.bank_rr % len(pool)]
        self.bank_rr += 1
        return b

    def mm(self, out, lhsT, rhs, start, stop, reads, writes):
        self.R.op("pe", lambda e: e.matmul(out, lhsT, rhs, start=start, stop=stop), reads, writes)

    def mmg(self, out, pairs, reads, writes, start=True, stop=True):
        pairs = list(pairs)

        def fn(e):
            ins = None
            n = len(pairs)
            for i, (l, r) in enumerate(pairs):
                ins = e.matmul(out, l, r, start=(start and i == 0), stop=(stop and i == n - 1))
            return ins
        self.R.op("pe", fn, reads, writes)

    def act(self, out, in_, func, reads, writes, scale=None, bias=None, accum_out=None):
        kw = {}
        if scale is not None:
            kw["scale"] = scale
        if bias is not None:
            kw["bias"] = bias
        if accum_out is not None:
            kw["accum_out"] = accum_out
        self.R.op("act", lambda e: e.activation(out=out, in_=in_, func=func, **kw), reads, writes)

    def tt(self, eng, out, in0, in1, op, reads, writes):
        self.R.op(eng, lambda e: e.tensor_tensor(out=out, in0=in0, in1=in1, op=op), reads, writes)

    def ts(self, eng, out, in0, s1, op0, reads, writes, s2=None, op1=None):
        if op1 is None:
            self.R.op(eng, lambda e: e.tensor_scalar(out=out, in0=in0, scalar1=s1, scalar2=None, op0=op0),
                      reads, writes)
        else:
            self.R.op(eng, lambda e: e.tensor_scalar(out=out, in0=in0, scalar1=s1, scalar2=s2, op0=op0, op1=op1),
                      reads, writes)

    def stt(self, out, in0, scalar, in1, op0, op1, reads, writes):
        self.R.op("dve", lambda e: e.scalar_tensor_tensor(out=out, in0=in0, scalar=scalar, in1=in1,
                                                           op0=op0, op1=op1), reads, writes)

    def cp(self, eng, out, in_, reads, writes):
        if eng == "act":
            self.act(out, in_, AF.Copy, reads, writes)
        else:
            self.R.op(eng, lambda e: e.tensor_copy(out=out, in_=in_), reads, writes)

    def dma(self, q, out, in_, sem, reads, writes, background=False):
        return self.R.dma(q, lambda e: e.dma_start(out=out, in_=in_), sem, reads, writes, background=background)


def build_program(debug=False, n_layers=DEPTH, stop_after=None, dbg_nexp=None, dbg_nblk=None):
    B = Builder(debug)
    nc, R = B.nc, B.R
    blks = token_blocks()

    x_d = B.din("x", [SEQ, D])
    meta_d = B.din("meta", [NMETA, D])
    embgb_d = B.din("embgb", [128, 2, 8])
    win_d = B.din("w_in_ext", [DEPTH, D, 4096])
    convw_d = B.din("convw", [DEPTH, 128, 4, 3])
    lam_d = B.din("lam", [DEPTH, 128, 4, 64])
    subg_d = B.din("subg", [DEPTH, 128, 1])
    wout_d = B.din("w_out", [DEPTH, D, D])
    lngb_d = B.din("lngb", [DEPTH, 128, 4, 8])
    wgd_d = B.din("w_gate_dense", [1, D, FD])
    wud_d = B.din("w_up_dense", [1, D, FD])
    wdd_d = B.din("w_down_dense", [1, FD, D])
    wr_d = B.din("w_router", [128, 8, NE])
    wgm_d = B.din("w_gate_moe", [1, NE, D, FE])
    wum_d = B.din("w_up_moe", [1, NE, D, FE])
    wdm_d = B.din("w_down_moe", [1, NE, FE, D])
    cos_d = B.din("cosT", [128, L])
    sin_d = B.din("sinT", [128, L])
    mask_d = B.din("maskb", [128, 4, 512], BF16)
    identf_d = B.din("identf", [128, 128])
    cb_d = B.din("constb", [128, 5, 128], BF16)
    sel_d = B.din("sel", [8, NE, 128])
    constf_d = B.din("constf", [128, 2, 128])
    gbrep_d = B.din("gbrep", [128, 2, D])
    y_d = B.dout("y", [SEQ, D])
    MTf = B.dscr("MTf", [SEQ, D], F32)
    MTb = B.dscr("MTb", [SEQ, D], BF16)
    Xg = B.dscr("Xg", [NITEM * 512, D], BF16)
    Ys = B.dscr("Ys", [NITEM * 512, D], F32)

    Hf = [B.dscr("Hf%d" % l, [128, 8, L], F32) for l in range(DEPTH)]
    Hb = [B.dscr("Hb%d" % l, [128, 8, L], BF16) for l in range(DEPTH)]
    Mf = [B.dscr("Mf%d" % l, [128, 8, L], F32) for l in range(DEPTH)]
    Mb = [B.dscr("Mb%d" % l, [128, 8, L], BF16) for l in range(DEPTH)]
    QS = [B.dscr("QS%d" % l, [128, 4, L], BF16) for l in range(DEPTH)]
    CO = [B.dscr("CO%d" % l, [128, 4, L], BF16) for l in range(DEPTH)]
    WGs = [B.nc.dram_tensor("WGs0", [1, D, FD], BF16).ap(), B.nc.dram_tensor("WGs1", [NE, D, FE], BF16).ap()]
    WUs = [B.nc.dram_tensor("WUs0", [1, D, FD], BF16).ap(), B.nc.dram_tensor("WUs1", [NE, D, FE], BF16).ap()]
    WDs = [B.nc.dram_tensor("WDs0", [1, FD, D], BF16).ap(), B.nc.dram_tensor("WDs1", [NE, FE, D], BF16).ap()]
    WG2, WU2, WD2 = WGs[1].rearrange("e d f -> (e d f)").rearrange("(r x) -> r x", x=4096), \
        WUs[1].rearrange("e d f -> (e d f)").rearrange("(r x) -> r x", x=4096), \
        WDs[1].rearrange("e f d -> (e f d)").rearrange("(r x) -> r x", x=4096)
    pconst_d = B.din("pconst", [128, 7])

    top = B.st
    ps = [top.enter_context(nc.psum_tensor("ps%d" % i, [128, 512], F32)) for i in range(8)]

    def PK(b):
        return ("ps", b)

    identf = B.sb(top, "identf", [128, 128], F32)
    cb = B.sb(top, "constb", [128, 5, 128], BF16)
    maskb = B.sb(top, "maskb", [128, 4, 512], BF16)
    embgb = B.sb(top, "embgb", [128, 2, 8], F32)
    lngb = B.sb(top, "lngb", [128, DEPTH, 4, 8], F32)
    convw = B.sb(top, "convw", [128, DEPTH, 4, 3], F32)
    lamt = B.sb(top, "lamt", [128, DEPTH, 4, 64], F32)
    subg = B.sb(top, "subg", [128, DEPTH, 1], F32)
    neglam = B.sb(top, "neglam", [128, DEPTH, 1], F32)
    gsc = B.sb(top, "gsc", [128, DEPTH, 1], F32)
    lamtmp = B.sb(top, "lamtmp", [128, 8], F32)
    lamprod = B.sb(top, "lamprod", [128, 2, 64], F32)
    wr = B.sb(top, "wr", [128, 8, NE], F32)
    sel = B.sb(top, "sel", [8, NE, 128], F32)
    constf = B.sb(top, "constf", [128, 2, 128], F32)
    LG = B.sb(top, "LG", [128, 32, NE], F32)
    B.dma("sp", constf[:], constf_d, "c0", [], ["constf"])
    epst = B.sb(top, "epst", [128, 1], F32)
    R.op("pool", lambda e: e.memset(epst[:], LN_EPS), [], ["epst"])
    identb = cb[:, 0, :]
    onesb = cb[:, 1, :]
    ones0b = cb[:, 2, :]
    onesM = cb[:, 3, :]
    ones128 = cb[:, 4, :]

    B.dma("sp", identf[:], identf_d, "c0", [], ["identf"])
    B.dma("sp", cb[:], cb_d, "c0", [], ["cb"])
    B.dma("sp", maskb[:], mask_d, "c0", [], ["maskb"])
    B.dma("sp", embgb[:], embgb_d, "c0", [], ["embgb"])
    for l in range(DEPTH):
        B.dma("sp", lngb[:, l], lngb_d[l], "c0", [], ["lngb"])
        B.dma("sp", convw[:, l], convw_d[l], "c0", [], ["convw"])
        B.dma("sp", lamt[:, l], lam_d[l], "c0", [], ["lamt"])
        B.dma("sp", subg[:, l], subg_d[l], "c0", [], ["subg"])
    B.dma("sp", wr[:], wr_d, "c0", [], ["wr"])
    B.dma("sp", sel[:], sel_d, "c0", [], ["sel"])

    def cast(name, dst, src):
        c = src.shape[-1]
        parts = 1
        while c // parts > 2048 or c % parts:
            parts += 1
        if len(src.shape) == 3:
            src = src.rearrange("e r (a b) -> (e r a) b", a=parts)
            dst = dst.rearrange("e r (a b) -> (e r a) b", a=parts)
        else:
            src = src.rearrange("r (a b) -> (r a) b", a=parts)
            dst = dst.rearrange("r (a b) -> (r a) b", a=parts)
        B.dma("pool", dst, src, ("cast", name), [], [("W", name)], background=True)

    WGd2 = WGs[0].rearrange("e d f -> (e d f)").rearrange("(r x) -> r x", x=2048)
    WUd2 = WUs[0].rearrange("e d f -> (e d f)").rearrange("(r x) -> r x", x=2048)
    WDd2 = WDs[0].rearrange("e f d -> (e f d)").rearrange("(r x) -> r x", x=2048)
    dense_cast_keys = []
    dense_cast_queue = []

    def issue_dense_casts():
        for nm, dst2, srcw in (("g", WGd2, wgd_d), ("u", WUd2, wud_d)):
            for kc in range(8):
                dst = dst2[:, kc * 256:(kc + 1) * 256].rearrange("(g p) c -> p g c", p=128)
                src = srcw[0, kc * 128:(kc + 1) * 128, :].rearrange("p (g c) -> p g c", c=256)
                dense_cast_keys.append(("W0", nm, kc))
                dense_cast_queue.append(lambda dst=dst, src=src, sem=("cast", nm + "0"), key=dense_cast_keys[-1]:
                                        B.dma("pool", dst, src, sem, [], [key], background=True))
        for c in range(2):
            dst = WDd2[:, c * 1024:(c + 1) * 1024].rearrange("(g p) d -> p g d", p=128)
            src = wdd_d[0].rearrange("(g c p) d -> c p g d", p=128, c=2)[c]
            dense_cast_keys.append(("W0", "d", c))
            dense_cast_queue.append(lambda dst=dst, src=src, key=dense_cast_keys[-1]:
                                    B.dma("pool", dst, src, ("cast", "d0"), [], [key], background=True))

    moe_cast_keys = []
    cast_queue = []

    def pump_casts(k):
        for _ in range(k):
            if cast_queue:
                cast_queue.pop(0)()

    def issue_moe_cast(h):
        if not SPARSE:
            cast("g1_%d" % h, WGs[1][2 * h:2 * h + 2], wgm_d[0, 2 * h:2 * h + 2])
            cast("u1_%d" % h, WUs[1][2 * h:2 * h + 2], wum_d[0, 2 * h:2 * h + 2])
            cast("d1_%d" % h, WDs[1][2 * h:2 * h + 2], wdm_d[0, 2 * h:2 * h + 2])
            return
        for e_ in (2 * h, 2 * h + 1):
            for nm, dst2, srcw in (("g", WG2, wgm_d), ("u", WU2, wum_d)):
                for kc in range(8):
                    dst = dst2[e_ * 896:(e_ + 1) * 896, kc * 512:(kc + 1) * 512].rearrange("(g p) c -> p g c", p=128)
                    src = srcw[0, e_, kc * 128:(kc + 1) * 128, :].rearrange("p (g c) -> p g c", c=512)
                    moe_cast_keys.append(("W2", nm, e_, kc))
                    cast_queue.append(lambda dst=dst, src=src, sem=("cast", "%s1_%d" % (nm, h)), key=moe_cast_keys[-1]:
                                      B.dma("pool", dst, src, sem, [], [key], background=True))
            for c in range(4):
                dst = WD2[e_ * 896:(e_ + 1) * 896, c * 1024:(c + 1) * 1024].rearrange("(g p) d -> p g d", p=128)
                src = wdm_d[0, e_].rearrange("(g c p) d -> c p g d", p=128, c=4)[c]
                moe_cast_keys.append(("W2", "d", e_, c))
                cast_queue.append(lambda dst=dst, src=src, sem=("cast", "d1_%d" % h), key=moe_cast_keys[-1]:
                                  B.dma("pool", dst, src, sem, [], [key], background=True))

    for l in range(n_layers):
        lam_init = 0.8 - 0.6 * math.exp(-0.3 * l)
        B.tt("dve", lamprod[:, 0, :], lamt[:, l, 0, :], lamt[:, l, 1, :], ALU.mult, ["lamt"], ["lamprod"])
        B.tt("dve", lamprod[:, 1, :], lamt[:, l, 2, :], lamt[:, l, 3, :], ALU.mult, ["lamt"], ["lamprod2"])
        R.op("dve", lambda e: e.reduce_sum(out=lamtmp[:, 0:1], in_=lamprod[:, 0, :], axis=mybir.AxisListType.X),
             ["lamprod"], ["lt0"])
        R.op("dve", lambda e: e.reduce_sum(out=lamtmp[:, 1:2], in_=lamprod[:, 1, :], axis=mybir.AxisListType.X),
             ["lamprod2"], ["lt1"])
        B.act(lamtmp[:, 2:4], lamtmp[:, 0:2], AF.Exp, ["lt0", "lt1"], ["lt2"])
        B.tt("dve", lamtmp[:, 4:5], lamtmp[:, 3:4], lamtmp[:, 2:3], ALU.subtract, ["lt2"], ["lt4"])
        B.ts("dve", neglam[:, l, :], lamtmp[:, 4:5], -lam_init, ALU.add, ["lt4"], [("neglam", l)])
        B.ts("dve", gsc[:, l, :], subg[:, l, :], 1.0 - lam_init, ALU.mult, ["subg"], [("gsc", l)])

    def ln_stats_dc(bm, bq, dc, n, x, xbs, xsqs):
        u = dc % 2
        B.cp("dve", xbs[:, u, :n], x[:, dc, :n], [("x", dc)], [("xbs", u)])
        B.act(xsqs[:, u, :n], x[:, dc, :n], AF.Square, [("x", dc)], [("xsqs", u)])
        B.mm(ps[bm][:, :n], onesM, xbs[:, u, :n], dc == 0, dc == 7, ["cb", ("xbs", u)], [PK(bm)])
        B.mm(ps[bq][:, :n], onesM, xsqs[:, u, :n], dc == 0, dc == 7, ["cb", ("xsqs", u)], [PK(bq)])

    def ln_finish(bm, bq, n, x, tmp, small, gcol, bcol, ob, ob_key):
        m2, var, rstd, nmr = (small[:, i, :n] for i in range(4))
        B.act(m2, ps[bm][:, :n], AF.Square, [PK(bm)], ["m2"])
        B.stt(var, ps[bq][:, :n], LN_EPS, m2, ALU.add, ALU.subtract, [PK(bq), "m2"], ["var"])
        B.act(var, var, AF.Ln, ["var"], ["var"])
        B.act(rstd, var, AF.Exp, ["var"], ["rstd"], scale=-0.5)
        B.stt(nmr, ps[bm][:, :n], -1.0, rstd, ALU.mult, ALU.mult, [PK(bm), "rstd"], ["nmr"])
        for dc in range(8):
            t = tmp[:, dc % 2, :n]
            tk = ("lnt", dc % 2)
            B.tt("dve", t, x[:, dc, :n], rstd, ALU.mult, [("x", dc), "rstd"], [tk])
            B.tt("pool", t, t, nmr, ALU.add, [tk, "nmr"], [tk])
            B.act(x[:, dc, :n], t, AF.Identity, [tk, "lngb"], [("x", dc)], scale=gcol(dc), bias=bcol(dc))
            if ob is not None:
                B.act(ob[:, dc, :n], t, AF.Identity, [tk, "lngb"], [(ob_key, dc)], scale=gcol(dc), bias=bcol(dc))

    with ExitStack() as es:
        NES = 4
        xt = [B.sb(es, "e_xt%d" % i, [128, D], F32) for i in range(NES)]
        xn = [B.sb(es, "e_xn%d" % i, [128, D], F32) for i in range(NES)]
        st6 = B.sb(es, "e_st6", [128, NES, 2, 6], F32)
        mv = B.sb(es, "e_mv", [128, NES, 4], F32)
        hfo = [B.sb(es, "e_hf%d" % i, [128, 8, 128], F32) for i in range(NES)]
        hbo = [B.sb(es, "e_hb%d" % i, [128, 8, 128], BF16) for i in range(NES)]
        issue_dense_casts()
        def e_geom(i):
            return i % NES, (NMETA if i == 0 else 128), (0 if i == 0 else NMETA + (i - 1) * 128)

        def e_stage1(i):
            s, r, t0 = e_geom(i)
            src = meta_d if i == 0 else x_d[(i - 1) * 128:i * 128, :]
            B.dma("sp", xt[s][:r, :], src, ("e_xt", s), [], [("e_xt", s)])
            for h in range(2):
                R.op("dve", lambda e, s=s, h=h, r=r: e.bn_stats(out=st6[:r, s, h, :], in_=xt[s][:r, h * 512:(h + 1) * 512]),
                     [("e_xt", s)], [("e_st", s, h)])
            R.op("dve", lambda e, s=s, r=r: e.bn_aggr(out=mv[:r, s, 0:2], in_=st6[:r, s].rearrange("p a b -> p (a b)")),
                 [("e_st", s, 0), ("e_st", s, 1)], [("e_mv", s)])
            B.ts("dve", mv[:r, s, 3:4], mv[:r, s, 1:2], LN_EPS, ALU.add, [("e_mv", s)], [("e_ve", s)])
            B.act(mv[:r, s, 3:4], mv[:r, s, 3:4], AF.Ln, [("e_ve", s)], [("e_ve", s)])
            B.act(mv[:r, s, 2:3], mv[:r, s, 3:4], AF.Exp, [("e_ve", s)], [("e_rs", s)], scale=-0.5)

        def e_stage2(i):
            s, r, t0 = e_geom(i)
            B.ts("dve", xn[s][:r, :], xt[s][:r, :], mv[:r, s, 0:1], ALU.subtract,
                 [("e_xt", s), ("e_mv", s), ("e_rs", s)], [("e_xn", s)], s2=mv[:r, s, 2:3], op1=ALU.mult)
            for half in range(2):
                bk = B.bank()

                def tr(e, s=s, r=r, half=half, bk=bk):
                    ins = None
                    for q in range(4):
                        dc = half * 4 + q
                        ins = e.transpose(ps[bk][:, q * 128:q * 128 + r], xn[s][:r, dc * 128:(dc + 1) * 128],
                                          identf[:r, :r])
                    return ins
                R.op("pe", tr, [("e_xn", s), "identf"], [PK(bk)])
                for q in range(4):
                    dc = half * 4 + q
                    B.act(hfo[s][:, dc, :r], ps[bk][:, q * 128:q * 128 + r], AF.Identity,
                          [PK(bk), "embgb"], [("e_hf", s, dc)], scale=embgb[:, 0, dc:dc + 1], bias=embgb[:, 1, dc:dc + 1])

        def e_stage3(i):
            s, r, t0 = e_geom(i)
            B.cp("pool", hbo[s][:, :, :r], hfo[s][:, :, :r], [("e_hf", s, dc) for dc in range(8)], [("e_hb", s)])
            B.dma("sp", Hf[0][:, :, t0:t0 + r], hfo[s][:, :, :r], ("e_of", s),
                  [("e_hf", s, dc) for dc in range(8)], [("Hf", 0, "t", i)])
            B.dma("sp", Hb[0][:, :, t0:t0 + r], hbo[s][:, :, :r], ("e_of", s), [("e_hb", s)], [("Hb", 0, "t", i)])

        for step in range(33 + 2):
            if step < 33:
                e_stage1(step)
            if 0 <= step - 1 < 33:
                e_stage2(step - 1)
            if 0 <= step - 2 < 33:
                e_stage3(step - 2)

    R.barrier()

    def hkeys(name, l, j):
        if l == 0:
            if j == 0:
                return [(name, 0, "t", 0)]
            return [(name, 0, "t", 4 * (j - 1) + 1 + q) for q in range(4)]
        return [(name, l, j)]

    final_ops = []
    moe_cast_next = [0]
    g01 = B.sb(top, "g01", [128, 2, 32], F32)
    gidx = B.sb(top, "gidx", [128, 2, 32], I32)
    eidx = B.sb(top, "eidx", [128, 32], I32)
    widx = B.sb(top, "widx", [128, NITEM, 7], I32)
    pconst = B.sb(top, "pconst", [128, 7], F32)
    B.dma("sp", pconst[:], pconst_d, "c0", [], ["pconst"])
    if debug:
        dbg_gidx = B.nc.dram_tensor("dbg_gidx", [128, 2, 32], I32, kind="ExternalOutput").ap()
        dbg_eidx = B.nc.dram_tensor("dbg_eidx", [128, 32], I32, kind="ExternalOutput").ap()
        dbg_g01 = B.nc.dram_tensor("dbg_g01", [128, 2, 32], F32, kind="ExternalOutput").ap()

    def sparse_moe_phase(l):
        AX = mybir.AxisListType.X
        FC = FE // 128
        GC = 512
        NG = FE // GC
        CPG = GC // 128
        DG = 4
        NDG = FC // DG
        castkeys = list(moe_cast_keys)
        onesf = constf[:, 0, :]
        utri = constf[:, 1, :]
        with ExitStack() as es:
            m8 = B.sb(es, "r_m8", [128, 32, NE], F32)
            selt = B.sb(es, "r_sel", [128, 32, NE], F32)
            top1 = B.sb(es, "r_top1", [128, 32, NE], F32)
            cum = B.sb(es, "r_cum", [128, 32, NE], F32)
            tot = B.sb(es, "r_tot", [128, 32, NE], F32)
            off = B.sb(es, "r_off", [128, 32, NE], F32)
            gs = B.sb(es, "r_gs", [128, 32, NE], F32)
            prod = B.sb(es, "r_prod", [128, 32, NE], F32)
            sm = B.sb(es, "r_sm", [128, 8, NE], F32)
            gk = B.sb(es, "r_gk", [128, 2, 32], F32)
            dd = B.sb(es, "r_dd", [128, 3, 32], F32)
            eid = B.sb(es, "r_eid", [128, 32], F32)
            for t in range(32):
                R.op("dve", lambda e, t=t: e.max(out=m8[:, t, :], in_=LG[:, t, :]), [("LG", t)], [("m8", t)])
            for t in range(32):
                B.ts("dve", selt[:, t, :], LG[:, t, :], m8[:, t, 1:2], ALU.is_ge, [("LG", t), ("m8", t)], ["r_sel"])
                B.ts("dve", top1[:, t, :], LG[:, t, :], m8[:, t, 0:1], ALU.is_ge, [("LG", t), ("m8", t)], ["r_top1"])
            m8k = [("m8", t) for t in range(32)]
            B.tt("dve", dd[:, 0, :], m8[:, :, 1], m8[:, :, 0], ALU.subtract, m8k, ["r_d0"])
            B.act(dd[:, 1, :], dd[:, 0, :], AF.Exp, ["r_d0"], ["r_d1"])
            B.ts("dve", dd[:, 2, :], dd[:, 1, :], 1.0, ALU.add, ["r_d1"], ["r_d2"])
            R.op("dve", lambda e: e.reciprocal(out=g01[:, 0, :], in_=dd[:, 2, :]), ["r_d2"], ["g0"])
            B.tt("dve", g01[:, 1, :], dd[:, 1, :], g01[:, 0, :], ALU.mult, ["r_d1", "g0"], ["g1"])
            selflat = selt[:].rearrange("p t e -> p (t e)")
            b1, b2 = B.bank(), B.bank()
            B.mm(ps[b1][:, 0:256], utri, selflat, True, True, ["constf", "r_sel"], [PK(b1)])
            B.mm(ps[b2][:, 0:256], onesf, selflat, True, True, ["constf", "r_sel"], [PK(b2)])
            B.cp("dve", cum[:].rearrange("p t e -> p (t e)"), ps[b1][:, 0:256], [PK(b1)], ["r_cum"])
            B.cp("act", tot[:].rearrange("p t e -> p (t e)"), ps[b2][:, 0:256], [PK(b2)], ["r_tot"])
            R.op("dve", lambda e: e.memset(off[:, 0, :], 0.0), [], [("r_off", 0)])
            for t in range(1, 32):
                B.tt("dve", off[:, t, :], off[:, t - 1, :], tot[:, t - 1, :], ALU.add, [("r_off", t - 1), "r_tot"], [("r_off", t)])
            cnt, ntile, ibase, iend, base5, tq = (sm[:, i, :] for i in range(6))
            B.tt("dve", cnt, off[:, 31, :], tot[:, 31, :], ALU.add, [("r_off", 31), "r_tot"], ["r_cnt"])
            B.ts("dve", ntile, cnt, 0.0, ALU.is_gt, ["r_cnt"], ["r_nt"])
            for m in range(1, 8):
                B.ts("dve", tq, cnt, 512.0 * m, ALU.is_gt, ["r_cnt"], ["r_tq"])
                B.tt("dve", ntile, ntile, tq, ALU.add, ["r_nt", "r_tq"], ["r_nt"])
            R.op("dve", lambda e: e.memset(ibase[:, 0:1], 0.0), [], [("r_ib", 0)])
            for e_ in range(1, NE):
                B.tt("dve", ibase[:, e_:e_ + 1], ibase[:, e_ - 1:e_], ntile[:, e_ - 1:e_], ALU.add,
                     [("r_ib", e_ - 1), "r_nt"], [("r_ib", e_)])
            ibk = [("r_ib", e_) for e_ in range(NE)]
            B.tt("dve", iend, ibase, ntile, ALU.add, ibk + ["r_nt"], ["r_ie"])
            B.ts("dve", base5, ibase, 512.0, ALU.mult, ibk, ["r_b5"])
            for t in range(32):
                B.tt("dve", off[:, t, :], off[:, t, :], base5, ALU.add, [("r_off", t), "r_b5"], [("r_off2", t)])
            offk = [("r_off", t) for t in range(32)] + [("r_off2", t) for t in range(32)]
            B.tt("dve", gs[:], cum[:], off[:], ALU.add, ["r_cum"] + offk, ["r_gs"])
            B.tt("dve", prod[:], top1[:], gs[:], ALU.mult, ["r_top1", "r_gs"], ["r_prod"])
            R.op("dve", lambda e: e.reduce_sum(out=gk[:, 0, :], in_=prod[:], axis=AX), ["r_prod"], ["r_gk0"])
            B.tt("dve", top1[:], selt[:], top1[:], ALU.subtract, ["r_sel", "r_top1"], ["r_top2"])
            B.tt("dve", prod[:], top1[:], gs[:], ALU.mult, ["r_top2", "r_gs", "r_gk0"], ["r_prod"])
            R.op("dve", lambda e: e.reduce_sum(out=gk[:, 1, :], in_=prod[:], axis=AX), ["r_prod"], ["r_gk1"])
            B.cp("dve", gidx[:], gk[:], ["r_gk0", "r_gk1"], ["gidx"])
            for w in range(NITEM):
                B.ts("dve", tq, iend, float(w), ALU.is_le, ["r_ie"], ["r_tq"])
                R.op("dve", lambda e, w=w: e.reduce_sum(out=eid[:, w:w + 1], in_=tq, axis=AX), ["r_tq"], [("r_eid", w)])
            eidk = [("r_eid", w) for w in range(NITEM)]
            B.ts("dve", eid[:, 0:NITEM], eid[:, 0:NITEM], float(NE - 1), ALU.min, eidk, ["r_eid2"], s2=896.0, op1=ALU.mult)
            B.cp("dve", eidx[:, 0:NITEM], eid[:, 0:NITEM], ["r_eid2"], ["eidx"])
            wif = B.sb(es, "r_wif", [128, NITEM, 7], F32)
            for w in range(NITEM):
                B.ts("dve", wif[:, w, :], pconst[:, :], eid[:, w:w + 1], ALU.add, ["r_eid2", "pconst"], [("r_wif", w)])
            B.cp("dve", widx[:], wif[:], [("r_wif", w) for w in range(NITEM)], ["widx"])
            if debug:
                B.dma("sp", dbg_gidx, gidx[:], "dbg1", ["gidx"], [])
                B.dma("sp", dbg_eidx, eidx[:], "dbg1", ["eidx"], [])
                B.dma("sp", dbg_g01, g01[:], "dbg1", ["g0", "g1"], [])
        R.barrier()
        if stop_after == ("R", l):
            return
        with ExitStack() as es:
            mt = [B.sb(es, "s_mt%d" % i, [128, D], BF16) for i in range(4)]
            xgz = [("Xgz", w_) for w_ in range(NITEM)]
            for tt in range(32):
                s = tt % 4
                B.dma("sp", mt[s][:], MTb[tt * 128:(tt + 1) * 128, :], ("s_mt", s), [("MTb", tt)], [("s_mt", s)])
                for k in range(2):
                    R.dma("pool", lambda e, s=s, k=k, tt=tt: e.indirect_dma_start(
                        out=Xg, out_offset=bass.IndirectOffsetOnAxis(ap=gidx[:, k, tt:tt + 1], axis=0),
                        in_=mt[s][:], in_offset=None), ("s_sc", s), [("s_mt", s), "gidx"] + xgz, [("Xgw", tt, k)])
        R.barrier()
        if stop_after == ("S", l):
            return
        with ExitStack() as es:
            NWB = 2
            wgu = [B.sb(es, "m_wgu%d" % i, [128, 2, 8, GC], BF16) for i in range(NWB)]
            wdt = [B.sb(es, "m_wd%d" % i, [128, DG, D], BF16) for i in range(NWB)]
            xgt = [B.sb(es, "m_xg%d" % i, [128, 4, D], BF16) for i in range(2)]
            xT = [B.sb(es, "m_xT%d" % i, [128, 8, 512], BF16) for i in range(2)]
            actt = B.sb(es, "m_act", [128, FC, 512], BF16)
            sg = [B.sb(es, "m_sg%d" % i, [128, 512], F32) for i in range(3)]
            ysb = [B.sb(es, "m_y%d" % i, [128, 4, D], F32) for i in range(2)]
            xgw = [("Xgw", tt, k) for tt in range(32) for k in range(2)]
            wc = dwc = sgc = 0
            def item_inputs(w):
                s = w % 2
                B.dma("sp", xgt[s][:], Xg[w * 512:(w + 1) * 512, :].rearrange("(q p) d -> p q d", p=128),
                      ("m_xg", s), xgw, [("m_xg", s)])
                for kp in range(4):
                    bk = B.bank()
                    psb = ps[bk][:].bitcast(BF16)

                    def trx(e, s=s, kp=kp, psb=psb):
                        ins = None
                        for k2 in range(2):
                            kc = 2 * kp + k2
                            for q in range(4):
                                ins = e.transpose(psb[:, k2 * 512 + q * 128:k2 * 512 + (q + 1) * 128],
                                                  xgt[s][:, q, kc * 128:(kc + 1) * 128], identb)
                        return ins
                    R.op("pe", trx, [("m_xg", s), "cb"], [PK(bk)])
                    for k2 in range(2):
                        kc = 2 * kp + k2
                        B.cp("act" if k2 == 0 else "dve", xT[s][:, kc, :], psb[:, k2 * 512:(k2 + 1) * 512],
                             [PK(bk)], [("m_xT", s, kc)])

            item_inputs(0)
            for w in range(NITEM):
                s = w % 2
                xTk = [("m_xT", s, kc) for kc in range(8)]

                def gath(out_ap, src2, g, w=w):
                    return lambda e: e.indirect_dma_start(
                        out=out_ap, out_offset=None, in_=src2,
                        in_offset=bass.IndirectOffsetOnAxis(ap=widx[:, w, g:g + 1], axis=0))
                for g in range(NG):
                    if g == 3 and w + 1 < NITEM:
                        item_inputs(w + 1)
                    wi = wc % NWB
                    wc += 1
                    R.dma("pool", gath(wgu[wi][:, 0, :, :].rearrange("p k c -> p (k c)"), WG2, g),
                          ("m_wgu", wi), ["widx"] + castkeys, [("m_wgu", wi)])
                    R.dma("pool", gath(wgu[wi][:, 1, :, :].rearrange("p k c -> p (k c)"), WU2, g),
                          ("m_wgu", wi), ["widx"] + castkeys, [("m_wgu", wi)])
                    for c in range(CPG):
                        fc = g * CPG + c
                        bg, bu = B.bank(), B.bank()
                        B.mmg(ps[bg][:, :], [(wgu[wi][:, 0, kc, c * 128:(c + 1) * 128], xT[s][:, kc, :]) for kc in range(8)],
                              [("m_wgu", wi)] + xTk, [PK(bg)])
                        B.mmg(ps[bu][:, :], [(wgu[wi][:, 1, kc, c * 128:(c + 1) * 128], xT[s][:, kc, :]) for kc in range(8)],
                              [("m_wgu", wi)] + xTk, [PK(bu)])
                        q_ = sgc % 3
                        sgc += 1
                        B.act(sg[q_][:, :], ps[bg][:, :], AF.Silu, [PK(bg)], [("m_sg", q_)])
                        B.tt("dve", actt[:, fc, :], ps[bu][:, :], sg[q_][:, :], ALU.mult,
                             [PK(bu), ("m_sg", q_)], [("m_act", fc)])
                for dg in range(NDG):
                    wi = dwc % NWB
                    dwc += 1
                    R.dma("pool", gath(wdt[wi][:].rearrange("p c d -> p (c d)"), WD2, dg),
                          ("m_wd", wi), ["widx"] + castkeys, [("m_wd", wi)])
                    for qh in range(8):
                        q, half = qh // 2, qh % 2
                        for c in range(DG):
                            fc = dg * DG + c
                            B.mm(ps[qh][:, :], actt[:, fc, q * 128:(q + 1) * 128], wdt[wi][:, c, half * 512:(half + 1) * 512],
                                 fc == 0, fc == FC - 1, [("m_wd", wi), ("m_act", fc)], [PK(qh)])
                for qh in range(8):
                    q, half = qh // 2, qh % 2
                    B.cp("act" if qh % 2 == 0 else "dve", ysb[s][:, q, half * 512:(half + 1) * 512], ps[qh][:, :],
                         [PK(qh)], [("m_y", s, qh)])
                B.bank_rr = 0
                B.dma("sp", Ys[w * 512:(w + 1) * 512, :].rearrange("(q p) d -> p q d", p=128), ysb[s][:], ("m_ys", s),
                      [("m_y", s, qh) for qh in range(8)], [("Ys", w)])
        R.barrier()
        if stop_after == ("I", l):
            return
        with ExitStack() as es:
            gb = B.sb(es, "f_gb", [128, 2, D], F32)
            B.dma("sp", gb[:], gbrep_d, "f_gb", [], ["f_gb"])
            NS = 4
            y0 = [B.sb(es, "f_y0_%d" % i, [128, D], F32) for i in range(NS)]
            y1 = [B.sb(es, "f_y1_%d" % i, [128, D], F32) for i in range(NS)]
            hm = [B.sb(es, "f_hm%d" % i, [128, D], F32) for i in range(NS)]
            yo = [B.sb(es, "f_yo%d" % i, [128, D], F32) for i in range(NS)]
            st6 = B.sb(es, "f_st6", [128, NS, 2, 6], F32)
            mv = B.sb(es, "f_mv", [128, NS, 4], F32)
            ysk = [("Ys", w) for w in range(NITEM)]
            def f_stage1(tt):
                s = tt % NS
                R.dma("pool", lambda e, s=s, tt=tt: e.indirect_dma_start(
                    out=y0[s][:], out_offset=None, in_=Ys,
                    in_offset=bass.IndirectOffsetOnAxis(ap=gidx[:, 0, tt:tt + 1], axis=0)),
                    ("f_g0", s), ysk + ["gidx"], [("f_y0", s)])
                R.dma("pool", lambda e, s=s, tt=tt: e.indirect_dma_start(
                    out=y1[s][:], out_offset=None, in_=Ys,
                    in_offset=bass.IndirectOffsetOnAxis(ap=gidx[:, 1, tt:tt + 1], axis=0)),
                    ("f_g1", s), ysk + ["gidx"], [("f_y1", s)])
                B.dma("sp", hm[s][:], MTf[tt * 128:(tt + 1) * 128, :], ("f_hm", s), [("MTf", tt)], [("f_hm", s)])
                B.act(hm[s][:], hm[s][:], AF.Copy, [("f_hm", s)], [("f_hm", s)], scale=ALPHA)

            def f_stage2(tt):
                s = tt % NS
                B.stt(y0[s][:], y0[s][:], g01[:, 0, tt:tt + 1], hm[s][:], ALU.mult, ALU.add,
                      [("f_y0", s), ("f_hm", s), "g0"], [("f_y0", s)])
                B.stt(y0[s][:], y1[s][:], g01[:, 1, tt:tt + 1], y0[s][:], ALU.mult, ALU.add,
                      [("f_y1", s), ("f_y0", s), "g1"], [("f_y0", s)])
                for h in range(2):
                    R.op("dve", lambda e, s=s, h=h: e.bn_stats(out=st6[:, s, h, :], in_=y0[s][:, h * 512:(h + 1) * 512]),
                         [("f_y0", s)], [("f_st", s, h)])
                R.op("dve", lambda e, s=s: e.bn_aggr(out=mv[:, s, 0:2], in_=st6[:, s].rearrange("p a b -> p (a b)")),
                     [("f_st", s, 0), ("f_st", s, 1)], [("f_mv", s)])
                B.ts("dve", mv[:, s, 3:4], mv[:, s, 1:2], LN_EPS, ALU.add, [("f_mv", s)], [("f_ve", s)])
                B.act(mv[:, s, 3:4], mv[:, s, 3:4], AF.Ln, [("f_ve", s)], [("f_ve", s)])
                B.act(mv[:, s, 2:3], mv[:, s, 3:4], AF.Exp, [("f_ve", s)], [("f_rs", s)], scale=-0.5)

            def f_stage3(tt):
                s = tt % NS
                B.ts("dve", yo[s][:], y0[s][:], mv[:, s, 0:1], ALU.subtract,
                     [("f_y0", s), ("f_mv", s), ("f_rs", s)], [("f_yo", s)], s2=mv[:, s, 2:3], op1=ALU.mult)
                B.tt("dve", yo[s][:], yo[s][:], gb[:, 0, :], ALU.mult, [("f_yo", s), "f_gb"], [("f_yo", s)])
                B.tt("pool", yo[s][:], yo[s][:], gb[:, 1, :], ALU.add, [("f_yo", s), "f_gb"], [("f_yo", s)])
                final_ops.append(B.dma("sp", y_d[tt * 128:(tt + 1) * 128, :], yo[s][:], ("c_y", s), [("f_yo", s)], []))

            for step in range(32 + 2):
                if step < 32:
                    f_stage1(step)
                if 0 <= step - 1 < 32:
                    f_stage2(step - 1)
                if 0 <= step - 2 < 32:
                    f_stage3(step - 2)

    for l in range(n_layers):
        last = (l == DEPTH - 1)
        moe = (l % 2 == 1)
        with ExitStack() as ls:
            KT = B.sb(ls, "KT", [128, 4, LP], BF16)
            V = B.sb(ls, "V", [128, 33, 512], BF16)
            if l == 0 or True:
                R.op("pool", lambda e, KT=KT: e.memset(KT[:, :, 0:128], 0.0), [], [("KT", 0)])
                R.op("pool", lambda e, V=V: e.memset(V[:, 0, :], 0.0), [], [("V", 0)])
            with ExitStack() as es:
                W = B.sb(es, "W_in", [128, 8, 4096], BF16)
                wsrc = win_d[l].rearrange("(kc p) n -> p kc n", p=128)
                for g in range(8):
                    B.dma("pool", W[:, :, g * 512:(g + 1) * 512], wsrc[:, :, g * 512:(g + 1) * 512],
                          ("W", g // 2), [], [("Win", g)])
                hbk = [B.sb(es, "a_hb%d" % i, [128, 8, 512], BF16) for i in range(2)]
                cs = [B.sb(es, "a_cs%d" % i, [128, 2, 512], F32) for i in range(1)] * 2
                uh = [B.sb(es, "a_uh%d" % i, [128, 512], F32) for i in range(2)]
                vb = [[B.sb(es, "a_vb%d_%d" % (cc, p), [128, 514], F32) for p in range(2)] for cc in range(4)]
                yy = [B.sb(es, "a_y%d" % i, [128, 512], F32) for i in range(2)]
                co = [B.sb(es, "a_co%d" % i, [128, 4, 512], BF16) for i in range(1)] * 2
                t1 = [B.sb(es, "a_t1_%d" % i, [128, 512], F32) for i in range(2)]
                t2 = [B.sb(es, "a_t2_%d" % i, [128, 512], F32) for i in range(2)]
                qc = [B.sb(es, "a_qc%d" % i, [128, 4, 512], BF16) for i in range(1)] * 2
                for cc in range(4):
                    R.op("pool", lambda e, cc=cc, vb=vb: e.memset(vb[cc][0][:, 0:2], 0.0), [], [("vbh", cc, 0)])
                rr = 0
                def load_hb(j):
                    t0, n, pp0 = blks[j]
                    s = j % 2
                    B.dma("sp", hbk[s][:, :, :n], Hb[l][:, :, t0:t0 + n], ("a_hb", s), hkeys("Hb", l, j), [("a_hb", s)])
                load_hb(0)
                for j, (t0, n, pp0) in enumerate(blks):
                    s = j % 2
                    if j + 1 < len(blks):
                        load_hb(j + 1)
                    if l == 0:
                        for _ in range(3):
                            if dense_cast_queue:
                                dense_cast_queue.pop(0)()
                    B.dma("sp", cs[s][:, 0, :n], cos_d[:, t0:t0 + n], ("a_cs", 0), [], [("a_cs", 0)])
                    B.dma("sp", cs[s][:, 1, :n], sin_d[:, t0:t0 + n], ("a_cs", 0), [], [("a_cs", 0)])
                    if l == 0 and j >= 2 and moe_cast_next[0] < 4 and not SPARSE:
                        issue_moe_cast(moe_cast_next[0])
                        moe_cast_next[0] += 1

                    def zmm(bk, g, c0, s=s, n=n):
                        B.mmg(ps[bk][:, :n], [(W[:, kc, g * 512 + c0:g * 512 + c0 + 128], hbk[s][:, kc, :n]) for kc in range(8)],
                              [("Win", g), ("a_hb", s)], [PK(bk)])
                    par = j % 2
                    for cc in range(4):
                        bb, bc, bh = B.bank(), B.bank(), B.bank()
                        zmm(bh, 2, cc * 128)
                        zmm(bc, 1, cc * 128)
                        zmm(bb, 0, cc * 128)
                        u = rr % 2
                        rr += 1
                        B.cp("act", uh[u][:, :n], ps[bh][:, :n], [PK(bh)], [("a_uh", u)])
                        B.tt("dve", vb[cc][par][:, 2:2 + n], ps[bc][:, :n], uh[u][:, :n], ALU.mult,
                             [PK(bc), ("a_uh", u)], [("vb", cc, par)])
                        B.ts("dve", yy[u][:, :n], vb[cc][par][:, 2:2 + n], convw[:, l, cc, 2:3], ALU.mult,
                             [("vb", cc, par), "convw"], [("a_y", u)])
                        B.stt(yy[u][:, :n], vb[cc][par][:, 1:1 + n], convw[:, l, cc, 1:2], yy[u][:, :n], ALU.mult, ALU.add,
                              [("vb", cc, par), ("vbh", cc, par), "convw", ("a_y", u)], [("a_y", u)])
                        B.stt(yy[u][:, :n], vb[cc][par][:, 0:n], convw[:, l, cc, 0:1], yy[u][:, :n], ALU.mult, ALU.add,
                              [("vb", cc, par), ("vbh", cc, par), "convw", ("a_y", u)], [("a_y", u)])
                        B.cp("pool", vb[cc][1 - par][:, 0:2], vb[cc][par][:, n:n + 2],
                             [("vb", cc, par), ("vbh", cc, par)], [("vbh", cc, 1 - par)])
                        B.tt("dve", co[s][:, cc, :n], ps[bb][:, :n], yy[u][:, :n], ALU.mult,
                             [PK(bb), ("a_y", u)], [("a_co", 0, cc)])
                    B.dma("sp", CO[l][:, :, t0:t0 + n], co[s][:, :, :n], "a_cos",
                          [("a_co", 0, cc) for cc in range(4)], [("CO", l, j)])
                    for which in range(2):
                        for hh in range(4):
                            b1, b2 = B.bank(), B.bank()
                            zmm(b1, 3 + which, hh * 128)
                            zmm(b2, 6 + which, hh * 128)
                            u = rr % 2
                            rr += 1
                            B.tt("dve", t1[u][:, :n], ps[b1][:, :n], cs[s][:, 0, :n], ALU.mult,
                                 [PK(b1), ("a_cs", 0)], [("a_t1", u)])
                            B.tt("dve", t2[u][:, :n], ps[b2][:, :n], cs[s][:, 1, :n], ALU.mult,
                                 [PK(b2), ("a_cs", 0)], [("a_t2", u)])
                            if which == 0:
                                B.tt("pool", qc[s][:, hh, :n], t1[u][:, :n], t2[u][:, :n], ALU.add,
                                     [("a_t1", u), ("a_t2", u)], [("a_qc", 0, hh)])
                            else:
                                B.tt("pool", KT[:, hh, pp0:pp0 + n], t1[u][:, :n], t2[u][:, :n], ALU.add,
                                     [("a_t1", u), ("a_t2", u)], [("KT", j, hh)] if j else [("KT", 0), ("KT", j, hh)])
                    B.dma("sp", QS[l][:, :, t0:t0 + n], qc[s][:, :, :n], "a_qs",
                          [("a_qc", 0, hh) for hh in range(4)], [("QS", l, j)])
                    nsub = 1 if j == 0 else 4
                    for sub in range(nsub):
                        r = NMETA if j == 0 else 128
                        ti = 0 if j == 0 else 4 * (j - 1) + 1 + sub
                        bk = B.bank()
                        B.mmg(ps[bk][:r, :], [(hbk[s][:, kc, sub * 128:sub * 128 + r], W[:, kc, 5 * 512:6 * 512]) for kc in range(8)],
                              [("Win", 5), ("a_hb", s)], [PK(bk)])
                        B.cp("act", V[:r, ti, :], ps[bk][:r, :], [PK(bk)], [("V", ti)])
            while l == 0 and dense_cast_queue:
                dense_cast_queue.pop(0)()
            R.barrier()
            if stop_after == ("A", l):
                break
            with ExitStack() as es:
                Wo = B.sb(es, "W_out", [128, 8, D], BF16)
                wsrc = wout_d[l].rearrange("(kc p) n -> p kc n", p=128)
                for g in range(2):
                    B.dma("pool", Wo[:, :, g * 512:(g + 1) * 512], wsrc[:, :, g * 512:(g + 1) * 512],
                          "Wo", [], [("Wout", g)])
                if l == 0:
                    while moe_cast_next[0] < 4:
                        issue_moe_cast(moe_cast_next[0])
                        moe_cast_next[0] += 1
                Qa = [B.sb(es, "b_qa%d" % i, [128, 4, 512], BF16) for i in range(2)]
                Qb = [B.sb(es, "b_qb%d" % i, [128, 4, 512], BF16) for i in range(2)]
                cat = [B.sb(es, "b_cat%d" % i, [128, 8, 512], BF16) for i in range(1)] * 2
                hfk = [B.sb(es, "b_hf%d" % i, [128, 8, 512], F32) for i in range(1)] * 2
                NPT = 4
                pt = [B.sb(es, "b_pt%d" % i, [128, 512], BF16) for i in range(NPT)]
                r12 = B.sb(es, "b_r12", [128, 2, 512], F32)
                oab = B.sb(es, "b_oab", [128, 2, 512], F32)
                ot = B.sb(es, "b_o", [128, 512], F32)
                osq = B.sb(es, "b_osq", [128, 512], BF16)
                rs = B.sb(es, "b_rs", [128, 512], F32)
                x = B.sb(es, "b_x", [128, 8, 512], F32)
                xbs = B.sb(es, "b_xbs", [128, 2, 512], BF16)
                xsqs = B.sb(es, "b_xsqs", [128, 2, 512], BF16)
                tmp = B.sb(es, "b_tmp", [128, 2, 512], F32)
                small = B.sb(es, "b_small", [128, 4, 512], F32)
                sp_last = SPARSE and last
                ob = [None if sp_last else B.sb(es, "b_ob%d" % i, [128, 8, 512], BF16) for i in range(1)]
                if sp_last:
                    mtf = [B.sb(es, "b_mtf%d" % i, [128, D], F32) for i in range(2)]
                    mtb = [B.sb(es, "b_mtb%d" % i, [128, D], BF16) for i in range(2)]
                mtcs = [0]
                pending_post = []
                zero_queue = []
                if SPARSE and l == 0:
                    zt = B.sb(es, "b_zero", [128, 4096], BF16)
                    R.op("pool", lambda e, zt=zt: e.memset(zt[:], 0.0), [], ["b_zero"])
                    for w_ in range(NITEM):
                        zero_queue.append(lambda w_=w_, zt=zt: B.dma(
                            "sp", Xg[w_ * 512:(w_ + 1) * 512, :].rearrange("(p a) d -> p (a d)", a=4), zt[:], "b_z",
                            ["b_zero"], [("Xgz", w_)]))
                for i in range(2):
                    R.op("pool", lambda e, i=i, Qa=Qa: e.memset(Qa[i][64:128, :, :], 0.0), [], [("b_qaz", i)])
                    R.op("pool", lambda e, i=i, Qb=Qb: e.memset(Qb[i][0:64, :, :], 0.0), [], [("b_qbz", i)])
                ACC = (0, 1, 2, 3)
                STP = (4, 5, 6, 7)
                ptc = 0
                def load_q(j):
                    t0, n, pp0 = blks[j]
                    s = j % 2
                    B.dma("sp", Qa[s][0:64, :, :n], QS[l][0:64, :, t0:t0 + n], ("b_q", s), [("QS", l, j)], [("b_qa", s)])
                    B.dma("sp", Qb[s][64:128, :, :n], QS[l][64:128, :, t0:t0 + n], ("b_q", s), [("QS", l, j)], [("b_qb", s)])
                load_q(1 if last else 0)
                for j, (t0, n, pp0) in enumerate(blks):
                    if last and j == 0:
                        continue
                    s = j % 2
                    if j + 1 < len(blks):
                        load_q(j + 1)
                    B.dma("sp", cat[s][:, 0:4, :n], CO[l][:, :, t0:t0 + n], "b_co", [("CO", l, j)],
                          [("b_cat", 0, c) for c in range(4)])
                    B.dma("sp", hfk[s][:, :, :n], Hf[l][:, :, t0:t0 + n], ("b_hf", 0), hkeys("Hf", l, j), [("b_hf", 0)])
                    tiles = [0] if j == 0 else [0] + list(range(1, 4 * j + 1))
                    diag = {0: 0} if j == 0 else {4 * (j - 1) + 1 + r: r for r in range(4)}

                    def tile_blk(i):
                        return 0 if i == 0 else (i - 1) // 4 + 1
                    units = [(hh, m, i) for hh in range(4) for m in range(2) for i in tiles]
                    LA = 3
                    info = {}
                    for idx in range(len(units) + LA):
                        if idx < len(units):
                            hh, m, i = units[idx]
                            if m == 0 and i == tiles[0] and l == 0 and j >= 1:
                                pump_casts(5)
                                if zero_queue:
                                    zero_queue.pop(0)()
                            bk = B.bank(STP)
                            Qm = Qa if m == 0 else Qb
                            qk = [("b_qa", s), ("b_qaz", s)] if m == 0 else [("b_qb", s), ("b_qbz", s)]
                            isd = i in diag
                            kk = [("KT", tile_blk(i), hh)] + ([("KT", 0)] if i == 0 else [])
                            B.mm(ps[bk][:, :n], KT[:, hh, i * 128:(i + 1) * 128], Qm[s][:, hh, :n], True, not isd,
                                 kk + qk, [PK(bk)])
                            if isd:
                                B.mm(ps[bk][:, :n], identb, maskb[:, diag[i], :n], False, True,
                                     ["cb", "maskb"], [PK(bk)])
                            p = ptc % NPT
                            ptc += 1
                            B.act(pt[p][:, :n], ps[bk][:, :n], AF.Exp, [PK(bk)], [("b_pt", p)], scale=0.125)
                            info[idx] = p
                        if idx >= LA:
                            hh, m, i = units[idx - LA]
                            p = info[idx - LA]
                            first = (i == tiles[0])
                            lastt = (i == tiles[-1])
                            sset = (2 * hh + m) % 2
                            bo, bs = ACC[2 * sset], ACC[2 * sset + 1]
                            B.mm(ps[bo][:, :n], V[:, i, hh * 128:(hh + 1) * 128], pt[p][:, :n], first, lastt,
                                 [("V", i), ("b_pt", p)] + ([("V", 0)] if i == 0 else []), [PK(bo)])
                            B.mm(ps[bs][:, :n], ones0b if i == 0 else onesb, pt[p][:, :n], first, lastt,
                                 ["cb", ("b_pt", p)], [PK(bs)])
                            if lastt:
                                B.cp("act", r12[:, m, :n], ps[bs][:, :n], [PK(bs)], [("b_r", m)])
                                B.cp("act", oab[:, m, :n], ps[bo][:, :n], [PK(bo)], [("b_oab", m)])
                                if m == 1:
                                    for m in range(2):
                                        R.op("dve", lambda e, m=m, n=n, r12=r12: e.reciprocal(out=r12[:, m, :n], in_=r12[:, m, :n]),
                                             [("b_r", m)], [("b_r", m)])
                                        B.tt("dve", oab[:, m, :n], oab[:, m, :n], r12[:, m, :n], ALU.mult,
                                             [("b_oab", m), ("b_r", m)], [("b_oab", m)])
                                    B.stt(ot[:, :n], oab[:, 1, :n], neglam[:, l, :], oab[:, 0, :n], ALU.mult, ALU.add,
                                          [("b_oab", 0), ("b_oab", 1), ("neglam", l)], ["b_o"])
                                    B.act(osq[:, :n], ot[:, :n], AF.Square, ["b_o"], ["b_osq"])
                                    bk = B.bank(STP)
                                    B.mm(ps[bk][:, :n], ones128, osq[:, :n], True, True, ["cb", "b_osq"], [PK(bk)])
                                    B.act(rs[:, :n], ps[bk][:, :n], AF.Ln, [PK(bk), "epst"], ["b_rs0"], bias=epst[:, 0:1])
                                    B.act(rs[:, :n], rs[:, :n], AF.Exp, ["b_rs0"], ["b_rs"], scale=-0.5)
                                    B.stt(cat[s][:, 4 + hh, :n], ot[:, :n], gsc[:, l, :], rs[:, :n], ALU.mult, ALU.mult,
                                          ["b_o", "b_rs", ("gsc", l)], [("b_cat", 0, 4 + hh)])
                                    if hh == 0:
                                        while pending_post:
                                            pending_post.pop(0)()
                    bm, bq = ACC[0], ACC[1]
                    for dc in range(8):
                        bk = B.bank(STP)
                        B.mmg(ps[bk][:, :n], [(Wo[:, kc, dc * 128:(dc + 1) * 128], cat[s][:, kc, :n]) for kc in range(8)],
                              [("Wout", dc // 4)] + [("b_cat", 0, c) for c in range(8)], [PK(bk)])
                        B.stt(x[:, dc, :n], hfk[s][:, dc, :n], ALPHA, ps[bk][:, :n], ALU.mult, ALU.add,
                              [("b_hf", 0), PK(bk)], [("x", dc)])
                        ln_stats_dc(bm, bq, dc, n, x, xbs, xsqs)
                    ln_finish(bm, bq, n, x, tmp, small,
                              lambda dc: lngb[:, l, 0, dc:dc + 1], lambda dc: lngb[:, l, 1, dc:dc + 1],
                              ob[0], "b_ob")
                    def post_block(j=j, t0=t0, n=n):
                        if not sp_last:
                            B.dma("sp", Mf[l][:, :, t0:t0 + n], x[:, :, :n], "b_sf", [("x", dc) for dc in range(8)], [("Mf", l, j)])
                            B.dma("sp", Mb[l][:, :, t0:t0 + n], ob[0][:, :, :n], "b_sf", [("b_ob", dc) for dc in range(8)], [("Mb", l, j)])
                        else:
                            for q in range(4):
                                tt = 4 * (j - 1) + q
                                xk = [("x", dc) for dc in range(8)]
                                import os as _os
                                if not _os.environ.get("NO_ROUTER"):
                                    br = B.bank(STP)
                                    B.mmg(ps[br][:, 0:NE], [(x[:, kc, q * 128:(q + 1) * 128], wr[:, kc, :]) for kc in range(8)],
                                          xk + ["wr"], [PK(br)])
                                    B.cp("dve", LG[:, tt, :], ps[br][:, 0:NE], [PK(br)], [("LG", tt)])
                                if _os.environ.get("NO_TR"):
                                    continue
                                ms = mtcs[0] % 2
                                mtcs[0] += 1
                                for half in range(2):
                                    bk = B.bank(STP)

                                    def trb(e, q=q, half=half, bk=bk):
                                        ins = None
                                        for qq in range(4):
                                            dc = half * 4 + qq
                                            ins = e.transpose(ps[bk][:, qq * 128:(qq + 1) * 128],
                                                              x[:, dc, q * 128:(q + 1) * 128], identf[:, :])
                                        return ins
                                    R.op("pe", trb, xk + ["identf"], [PK(bk)])
                                    B.cp("act", mtf[ms][:, half * 512:(half + 1) * 512], ps[bk][:, :], [PK(bk)], [("b_mtf", ms, half)])
                                    B.cp("dve", mtb[ms][:, half * 512:(half + 1) * 512], ps[bk][:, :], [PK(bk)], [("b_mtb", ms, half)])
                                B.dma("sp", MTf[tt * 128:(tt + 1) * 128, :], mtf[ms][:], ("b_smf", ms),
                                      [("b_mtf", ms, 0), ("b_mtf", ms, 1)], [("MTf", tt)])
                                B.dma("sp", MTb[tt * 128:(tt + 1) * 128, :], mtb[ms][:], ("b_smb", ms),
                                      [("b_mtb", ms, 0), ("b_mtb", ms, 1)], [("MTb", tt)])
                    pending_post.append(post_block)
                while pending_post:
                    pending_post.pop(0)()
                while zero_queue:
                    zero_queue.pop(0)()
            R.barrier()
            if stop_after == ("B", l):
                break
        if SPARSE and moe:
            sparse_moe_phase(l)
            R.barrier()
            continue
        with ExitStack() as es:
            F = FE if moe else FD
            nexp = NE if moe else 1
            if moe and dbg_nexp is not None:
                nexp = dbg_nexp
            FC = F // 128
            GC = 512 if moe else 256
            NG = F // GC
            CPG = GC // 128
            DG = 4 if moe else 2
            NDG = FC // DG
            lw = 1 if moe else 0
            NWB = 2
            assert not moe, "dense-all-experts MoE path retired: use SPARSE"
            NWB = 3
            wgu = [B.sb(es, "c_wgu%d" % i, [128, 2, 8, GC], BF16) for i in range(NWB)]
            wdt = [B.sb(es, "c_wd%d" % i, [128, DG, D], BF16) for i in range(NWB)]
            NHB = 1 if moe else 2
            hmb = [B.sb(es, "c_hmb%d" % i, [128, 8, 512], BF16) for i in range(NHB)]
            hmf = [B.sb(es, "c_hmf%d" % i, [128, 8, 512], F32) for i in range(NHB)]
            actt = B.sb(es, "c_act", [128, FC, 512], BF16)
            sg = [B.sb(es, "c_sg%d" % i, [128, 512], F32) for i in range(3)]
            x = B.sb(es, "c_x", [128, 8, 512], F32)
            xbs = B.sb(es, "c_xbs", [128, 2, 512], BF16)
            xsqs = B.sb(es, "c_xsqs", [128, 2, 512], BF16)
            tmp = B.sb(es, "c_tmp", [128, 2, 512], F32)
            small = B.sb(es, "c_small", [128, 4, 512], F32)
            of = x
            ob = None if last else B.sb(es, "c_ob", [128, 8, 512], BF16)
            if moe:
                acc = x
                G = B.sb(es, "c_G", [128, NE, 512], F32)
                lts = B.sb(es, "c_lts", [8, 512], F32)
                m12 = B.sb(es, "c_m12", [128, 4, 512], F32)
            if last:
                yt = [B.sb(es, "c_yt%d" % i, [128, D], F32) for i in range(2)]
            wc = 0
            dwc = 0
            sgc = 0
            ytc = 0
            pending_c = []
            for j, (t0, n, pp0) in enumerate(blks):
                if last and j == 0:
                    continue
                if moe and dbg_nblk is not None and j > dbg_nblk:
                    continue
                s = j % NHB

                def load_hm(j):
                    t0, n, pp0 = blks[j]
                    s = j % NHB
                    B.dma("sp", hmb[s][:, :, :n], Mb[l][:, :, t0:t0 + n], ("c_hmb", s), [("Mb", l, j)], [("c_hmb", s)])
                    B.dma("sp", hmf[s][:, :, :n], Mf[l][:, :, t0:t0 + n], ("c_hmf", s), [("Mf", l, j)], [("c_hmf", s)])
                if j == 0:
                    load_hm(0)
                if j + 1 < len(blks):
                    load_hm(j + 1)
                if moe:
                    bk = B.bank()
                    B.mmg(ps[bk][:8, :n], [(wr[:, kc, :], hmf[s][:, kc, :n]) for kc in range(8)],
                          ["wr", ("c_hmf", s)], [PK(bk)])
                    B.cp("act", lts[:, :n], ps[bk][:8, :n], [PK(bk)], ["c_lts"])
                    for e_ in range(NE):
                        bk = B.bank()
                        B.mm(ps[bk][:, :n], sel[:, e_, :], lts[:, :n], True, True, ["sel", "c_lts"], [PK(bk)])
                        B.cp("act", G[:, e_, :n], ps[bk][:, :n], [PK(bk)], [("c_G", e_)])
                    m1, m2, tq, dd = (m12[:, i, :n] for i in range(4))
                    B.tt("dve", m1, G[:, 0, :n], G[:, 1, :n], ALU.max, [("c_G", 0), ("c_G", 1)], ["c_m1"])
                    B.tt("dve", m2, G[:, 0, :n], G[:, 1, :n], ALU.min, [("c_G", 0), ("c_G", 1)], ["c_m2"])
                    for e_ in range(2, NE):
                        B.tt("dve", tq, m1, G[:, e_, :n], ALU.min, ["c_m1", ("c_G", e_)], ["c_tq"])
                        B.tt("dve", m2, m2, tq, ALU.max, ["c_m2", "c_tq"], ["c_m2"])
                        B.tt("dve", m1, m1, G[:, e_, :n], ALU.max, ["c_m1", ("c_G", e_)], ["c_m1"])
                    B.tt("dve", dd, m2, m1, ALU.subtract, ["c_m1", "c_m2"], ["c_dd"])
                    B.act(dd, dd, AF.Exp, ["c_dd"], ["c_dd"])
                    B.ts("dve", dd, dd, 1.0, ALU.add, ["c_dd"], ["c_dd"])
                    R.op("dve", lambda e, dd=dd: e.reciprocal(out=dd, in_=dd), ["c_dd"], ["c_dd"])
                    for e_ in range(NE):
                        B.tt("dve", tq, G[:, e_, :n], m2, ALU.is_ge, [("c_G", e_), "c_m2"], ["c_tq"])
                        B.tt("dve", G[:, e_, :n], G[:, e_, :n], m1, ALU.subtract, [("c_G", e_), "c_m1"], [("c_G", e_)])
                        B.act(G[:, e_, :n], G[:, e_, :n], AF.Exp, [("c_G", e_)], [("c_G", e_)])
                        B.tt("dve", G[:, e_, :n], G[:, e_, :n], tq, ALU.mult, [("c_G", e_), "c_tq"], [("c_G", e_)])
                        B.tt("dve", G[:, e_, :n], G[:, e_, :n], dd, ALU.mult, [("c_G", e_), "c_dd"], [("c_G", e_)])
                for e_ in range(nexp):
                    gname, uname, dname = ("g1_%d" % (e_ // 2), "u1_%d" % (e_ // 2), "d1_%d" % (e_ // 2)) if moe else ("g0", "u0", "d0")
                    for g in range(NG):
                        if l == 0:
                            pump_casts(1)
                        if g == 2:
                            while pending_c:
                                pending_c.pop(0)()
                        w = wc % NWB
                        wc += 1
                        B.dma("sp", wgu[w][:, 0].rearrange("p k c -> p (k c)"), WGd2[g * 128:(g + 1) * 128, :],
                              ("c_wgu", w), dense_cast_keys, [("c_wgu", w)])
                        B.dma("sp", wgu[w][:, 1].rearrange("p k c -> p (k c)"), WUd2[g * 128:(g + 1) * 128, :],
                              ("c_wgu", w), dense_cast_keys, [("c_wgu", w)])
                        for c in range(CPG):
                            fc = g * CPG + c
                            bg, bu = B.bank(), B.bank()
                            B.mmg(ps[bg][:, :n], [(wgu[w][:, 0, kc, c * 128:(c + 1) * 128], hmb[s][:, kc, :n]) for kc in range(8)],
                                  [("c_wgu", w), ("c_hmb", s)], [PK(bg)])
                            B.mmg(ps[bu][:, :n], [(wgu[w][:, 1, kc, c * 128:(c + 1) * 128], hmb[s][:, kc, :n]) for kc in range(8)],
                                  [("c_wgu", w), ("c_hmb", s)], [PK(bu)])
                            q = sgc % 3
                            sgc += 1
                            B.act(sg[q][:, :n], ps[bg][:, :n], AF.Silu, [PK(bg)], [("c_sg", q)])
                            B.tt("dve", actt[:, fc, :n], ps[bu][:, :n], sg[q][:, :n], ALU.mult,
                                 [PK(bu), ("c_sg", q)], [("c_act", fc)])
                    for dg in range(NDG):
                        w = dwc % NWB
                        dwc += 1
                        B.dma("sp", wdt[w][:].rearrange("p c d -> p (c d)"), WDd2[dg * 128:(dg + 1) * 128, :],
                              ("c_wd", w), dense_cast_keys, [("c_wd", w)])
                        for dc in range(8):
                            for c in range(DG):
                                fc = dg * DG + c
                                B.mm(ps[dc][:, :n], wdt[w][:, c, dc * 128:(dc + 1) * 128], actt[:, fc, :n],
                                     fc == 0, fc == FC - 1, [("c_wd", w), ("c_act", fc)], [PK(dc)])
                    for dc in range(8):
                        if not moe:
                            B.stt(x[:, dc, :n], hmf[s][:, dc, :n], ALPHA, ps[dc][:, :n], ALU.mult, ALU.add,
                                  [("c_hmf", s), PK(dc)], [("x", dc)])
                        elif e_ == 0:
                            B.tt("dve", acc[:, dc, :n], ps[dc][:, :n], G[:, 0, :n], ALU.mult,
                                 [PK(dc), ("c_G", 0)], [("x", dc)])
                        else:
                            t = tmp[:, dc % 2, :n]
                            tk = ("lnt", dc % 2)
                            B.tt("dve", t, ps[dc][:, :n], G[:, e_, :n], ALU.mult, [PK(dc), ("c_G", e_)], [tk])
                            B.tt("pool", acc[:, dc, :n], acc[:, dc, :n], t, ALU.add, [("x", dc), tk], [("x", dc)])
                B.bank_rr = 0
                for dc in range(8):
                    if moe:
                        B.stt(x[:, dc, :n], hmf[s][:, dc, :n], ALPHA, x[:, dc, :n], ALU.mult, ALU.add,
                              [("c_hmf", s), ("x", dc)], [("x", dc)])
                def c_tail(j=j, t0=t0, n=n):
                    bm, bq = B.bank(), B.bank()
                    for dc in range(8):
                        ln_stats_dc(bm, bq, dc, n, x, xbs, xsqs)
                    ln_finish(bm, bq, n, x, tmp, small,
                              lambda dc: lngb[:, l, 2, dc:dc + 1], lambda dc: lngb[:, l, 3, dc:dc + 1],
                              ob, "c_ob")
                    B.dma("sp", Hf[l + 1][:, :, t0:t0 + n], of[:, :, :n], "c_sf", [("x", dc) for dc in range(8)], [("Hf", l + 1, j)])
                    B.dma("sp", Hb[l + 1][:, :, t0:t0 + n], ob[:, :, :n], "c_sf", [("c_ob", dc) for dc in range(8)], [("Hb", l + 1, j)])
                if not last:
                    pending_c.append(c_tail)
                else:
                    raise AssertionError("dense path is only used for non-final layers")
                    for sub in range(4):
                        q = ytc % 2
                        ytc += 1
                        for half in range(2):
                            bk = B.bank()

                            def tr(e, sub=sub, half=half, bk=bk):
                                ins = None
                                for qq in range(4):
                                    dc = half * 4 + qq
                                    ins = e.transpose(ps[bk][:, qq * 128:(qq + 1) * 128],
                                                      of[:, dc, sub * 128:(sub + 1) * 128], identf[:, :])
                                return ins
                            R.op("pe", tr, [("x", half * 4 + qq) for qq in range(4)] + ["identf"], [PK(bk)])
                            B.cp("act" if half == 0 else "dve", yt[q][:, half * 512:(half + 1) * 512], ps[bk][:, :],
                                 [PK(bk)], [("c_yt", q, half)])
                        row0 = t0 - NMETA + sub * 128
                        final_ops.append(B.dma("sp", y_d[row0:row0 + 128, :], yt[q][:], ("c_y", q),
                                               [("c_yt", q, 0), ("c_yt", q, 1)], []))
            while pending_c:
                pending_c.pop(0)()
        pump_casts(len(cast_queue))
        R.barrier()
        if stop_after == ("C", l):
            break

    R.emit(nc, final_wait_ops=sorted({op.sem for op in final_ops}))
    return B


def _consts():
    inv = (1.0 / (np.float32(10000.0) ** (np.arange(0, 64, 2, dtype=np.float32) / np.float32(64)))).astype(np.float32)
    ang = np.arange(L, dtype=np.float32)[:, None] * inv[None, :]
    ang = np.concatenate([ang, ang], axis=-1).astype(np.float32)
    cos = np.cos(ang).astype(np.float32).T
    sin = np.sin(ang).astype(np.float32).T
    sgn = np.where(np.arange(64) < 32, -1.0, 1.0).astype(np.float32)[:, None]
    cosT = np.concatenate([cos, cos], axis=0)
    sinT = np.concatenate([sin * sgn, sin * sgn], axis=0)
    p = np.arange(128)[:, None, None]
    r = np.arange(4)[None, :, None]
    f = np.arange(512)[None, None, :]
    maskb = np.where(f >= 128 * r + p, 0.0, NEG).astype(ml_dtypes.bfloat16)
    identf = np.eye(128, dtype=np.float32)
    cb = np.zeros((128, 5, 128), np.float32)
    cb[:, 0, :] = np.eye(128)
    cb[:, 1, :] = 1.0
    cb[:16, 2, :] = 1.0
    cb[:, 3, :] = 1.0 / 1024.0
    cb[:, 4, :] = 1.0 / 128.0
    sel = np.zeros((8, NE, 128), np.float32)
    for e in range(NE):
        sel[e, e, :] = 1.0
    pconst = (np.arange(7)[None, :] * 128 + np.arange(128)[:, None]).astype(np.float32)
    constf = np.zeros((128, 2, 128), np.float32)
    constf[:, 0, :] = 1.0
    constf[:, 1, :] = (np.arange(128)[:, None] < np.arange(128)[None, :]).astype(np.float32)
    return dict(cosT=np.ascontiguousarray(cosT), sinT=np.ascontiguousarray(sinT), maskb=maskb,
                identf=identf, constb=cb.astype(ml_dtypes.bfloat16), sel=sel, constf=constf, pconst=pconst)


def _fm(v):
    return np.ascontiguousarray(np.asarray(v, np.float32).reshape(8, 128).T)


def _prep_shared(inp):
    w_in = np.asarray(inp["w_in"], np.float32)
    perm = np.concatenate([np.arange(32, 64), np.arange(0, 32)])
    qcols = w_in[:, :, 1536:2048].reshape(DEPTH, D, 8, 64)[:, :, :, perm].reshape(DEPTH, D, 512)
    kcols = w_in[:, :, 2048:2560].reshape(DEPTH, D, 8, 64)[:, :, :, perm].reshape(DEPTH, D, 512)
    w_in_ext = np.ascontiguousarray(np.concatenate([w_in, qcols, kcols], axis=2))
    conv_w = np.asarray(inp["conv_w"], np.float32)
    convw = np.ascontiguousarray(conv_w.reshape(DEPTH, 3, 4, 128).transpose(0, 3, 2, 1))
    lam = np.stack([np.asarray(inp[k], np.float32) for k in ("lambda_q1", "lambda_k1", "lambda_q2", "lambda_k2")], axis=1)
    lam = np.ascontiguousarray(np.broadcast_to(lam[:, None], (DEPTH, 128, 4, 64)))
    subg = np.ascontiguousarray(np.asarray(inp["subln_g"], np.float32).reshape(DEPTH, 128, 1))
    lngb = np.stack([np.stack([_fm(inp[k][l]) for k in ("ln_mix_g", "ln_mix_b", "ln_ffn_g", "ln_ffn_b")], axis=1)
                     for l in range(DEPTH)], axis=0)
    embgb = np.stack([_fm(inp["ln_emb_g"]), _fm(inp["ln_emb_b"])], axis=1)
    wr = np.ascontiguousarray(np.asarray(inp["w_router"], np.float32)[0].reshape(8, 128, NE).transpose(1, 0, 2))
    gbrep = np.ascontiguousarray(np.broadcast_to(
        np.stack([np.asarray(inp["ln_ffn_g"], np.float32)[DEPTH - 1], np.asarray(inp["ln_ffn_b"], np.float32)[DEPTH - 1]], axis=0)[None],
        (128, 2, D)))
    sh = dict(
        gbrep=gbrep, meta=np.ascontiguousarray(np.asarray(inp["meta_tokens"], np.float32)),
        embgb=np.ascontiguousarray(embgb), w_in_ext=w_in_ext, convw=convw, lam=lam, subg=subg,
        w_out=np.ascontiguousarray(np.asarray(inp["w_out"], np.float32)), lngb=np.ascontiguousarray(lngb),
        w_gate_dense=np.asarray(inp["w_gate_dense"], np.float32), w_up_dense=np.asarray(inp["w_up_dense"], np.float32),
        w_down_dense=np.asarray(inp["w_down_dense"], np.float32), w_router=wr,
        w_gate_moe=np.asarray(inp["w_gate_moe"], np.float32), w_up_moe=np.asarray(inp["w_up_moe"], np.float32),
        w_down_moe=np.asarray(inp["w_down_moe"], np.float32),
    )
    sh.update(_consts())
    return sh


def kernel(**inputs):
    x = np.asarray(inputs["x"], np.float32)
    sh = _prep_shared(inputs)
    B = build_program(debug=False)
    in_maps = []
    for c in range(8):
        m = dict(sh)
        m["x"] = np.ascontiguousarray(x[c])
        in_maps.append(m)
    res = run_bass_kernel_spmd(B.nc, in_maps, core_ids=list(range(8)))
    return np.stack([np.asarray(r["y"], np.float32) for r in res.results], axis=0)
```
